# Optimizing a Trainium2 kernel written in Bass

```python
import math
import jax, jax.numpy as jnp
from jax import lax
import numpy as np

D_MODEL = 1024
BATCH = 16
SEQ = 2048
DEPTH = 2

PLE_DIM = 256
NORM_EPS = 1e-6

SSD_HEADS = 8
SSD_HEAD_DIM = 64
SSD_WIDTH = SSD_HEADS * SSD_HEAD_DIM
SSD_GROUPS = 2
SSD_STATE = 128
SSD_CONV = 4
SSD_CHUNK = 128
SSD_CONV_CH = SSD_WIDTH + 2 * SSD_GROUPS * SSD_STATE

DIFF_HEADS = 4
DIFF_QK_DIM = 32
DIFF_V_DIM = 2 * DIFF_QK_DIM
DIFF_QK_WIDTH = DIFF_HEADS * 2 * DIFF_QK_DIM
DIFF_WIDTH = DIFF_HEADS * DIFF_V_DIM
Q_BLOCK = 128
REL_BUCKETS = 32
REL_MAX_DIST = 128

MLSTM_HEADS = 4
MLSTM_HEAD_DIM = 64
MLSTM_WIDTH = MLSTM_HEADS * MLSTM_HEAD_DIM
MLSTM_CHUNK = 128

MIX_WIDTH = SSD_WIDTH + DIFF_WIDTH + MLSTM_WIDTH
FFN_HIDDEN = 256 * (-(-(8 * D_MODEL) // (3 * 256)))

IN_SPLITS = (SSD_WIDTH, SSD_CONV_CH, SSD_HEADS,
             DIFF_QK_WIDTH, DIFF_QK_WIDTH, DIFF_WIDTH,
             MLSTM_WIDTH, MLSTM_WIDTH, MLSTM_WIDTH, MLSTM_WIDTH, MLSTM_HEADS, MLSTM_HEADS)
IN_COLS = sum(IN_SPLITS)

kernel_name = "hybrid_ssd_diffattn_mlstm_trunk"


def rmsnorm(x, w):
    xf = x.astype(jnp.float32)
    var = jnp.mean(xf * xf, axis=-1, keepdims=True)
    return (xf * lax.rsqrt(var + NORM_EPS)).astype(x.dtype) * w


def split_cols(t, sizes):
    offs = []
    acc = 0
    for s in sizes[:-1]:
        acc += s
        offs.append(acc)
    return jnp.split(t, offs, axis=-1)


def causal_depthwise_conv(x, w, b):
    k = w.shape[0]
    c = x.shape[-1]
    y = lax.conv_general_dilated(x, w[:, None, :], window_strides=(1,), padding=[(k - 1, 0)],
                                 dimension_numbers=('NWC', 'WIO', 'NWC'), feature_group_count=c)
    return y + b


def segsum_exp(a):
    n = a.shape[-1]
    cs = jnp.cumsum(a, axis=-1)
    diff = cs[..., :, None] - cs[..., None, :]
    mask = jnp.tril(jnp.ones((n, n), dtype=bool))
    return jnp.where(mask, jnp.exp(jnp.where(mask, diff, 0.0)), 0.0)


def ssd_mixer(z, xbc, dt_raw, conv_w, conv_b, dt_bias, a_log, d_skip, norm_w):
    b_, s_, _ = xbc.shape
    dtp = xbc.dtype
    G, HG, P, N, L = SSD_GROUPS, SSD_HEADS // SSD_GROUPS, SSD_HEAD_DIM, SSD_STATE, SSD_CHUNK
    nc = s_ // L
    xbc = jax.nn.silu(causal_depthwise_conv(xbc, conv_w, conv_b))
    xs, bm, cm = jnp.split(xbc, [SSD_WIDTH, SSD_WIDTH + G * N], axis=-1)
    dt = jax.nn.softplus(dt_raw.astype(jnp.float32) + dt_bias.astype(jnp.float32))
    a = -jnp.exp(a_log.astype(jnp.float32))
    a_dt = dt * a
    x = xs.reshape(b_, nc, L, G, HG, P)
    xdt = x * dt.reshape(b_, nc, L, G, HG)[..., None].astype(dtp)
    bmc = bm.reshape(b_, nc, L, G, N)
    cmc = cm.reshape(b_, nc, L, G, N)
    a_c = a_dt.reshape(b_, nc, L, G, HG).transpose(0, 3, 4, 1, 2)
    a_cs = jnp.cumsum(a_c, axis=-1)
    decay = segsum_exp(a_c)
    cb = jnp.einsum('bclgn,bcsgn->bgcls', cmc, bmc)
    y_diag = jnp.einsum('bghcls,bcsghp->bclghp', cb[:, :, None] * decay.astype(dtp), xdt)
    dstate = jnp.exp(a_cs[..., -1:] - a_cs).astype(dtp)
    states = jnp.einsum('bclgn,bghcl,bclghp->bcghpn', bmc, dstate, xdt)
    chunk_decay = jnp.exp(a_cs[..., -1]).astype(dtp)

    def step(carry, inp):
        st, dec = inp
        return carry * dec[..., None, None] + st, carry

    _, prev = lax.scan(step, jnp.zeros_like(states[:, 0]),
                       (jnp.moveaxis(states, 1, 0), jnp.moveaxis(chunk_decay, 3, 0)))
    prev = jnp.moveaxis(prev, 0, 1)
    y_off = jnp.einsum('bclgn,bcghpn,bghcl->bclghp', cmc, prev, jnp.exp(a_cs).astype(dtp))
    y = (y_diag + y_off).reshape(b_, s_, G, HG, P) + x.reshape(b_, s_, G, HG, P) * d_skip.reshape(G, HG)[:, :, None]
    y = y.reshape(b_, s_, SSD_WIDTH)
    return rmsnorm(y * jax.nn.silu(z), norm_w)


def t5_causal_bucket(dist):
    max_exact = REL_BUCKETS // 2
    is_small = dist < max_exact
    d = jnp.maximum(dist, 1).astype(jnp.float32)
    large = max_exact + (jnp.log(d / max_exact) / math.log(REL_MAX_DIST / max_exact)
                         * (REL_BUCKETS - max_exact)).astype(jnp.int32)
    large = jnp.minimum(large, REL_BUCKETS - 1)
    return jnp.where(is_small, dist, large)


def diff_attention(q, k, v, lam, lam_init, rel_bias, norm_w):
    b_, s_, h_, _, d = q.shape
    scale = d ** -0.5
    outs = []
    for j in range(s_ // Q_BLOCK):
        q0 = j * Q_BLOCK
        kl = q0 + Q_BLOCK
        qb = q[:, q0:kl]
        kb = k[:, :kl]
        vb = v[:, :kl]
        sc = jnp.einsum('bqhmd,bkhmd->bhmqk', qb, kb).astype(jnp.float32) * scale
        dist = (q0 + jnp.arange(Q_BLOCK, dtype=jnp.int32))[:, None] - jnp.arange(kl, dtype=jnp.int32)[None, :]
        bias = rel_bias[t5_causal_bucket(jnp.maximum(dist, 0))]
        sc = sc + jnp.transpose(bias, (2, 0, 1))[None, :, None].astype(jnp.float32)
        sc = jnp.where(dist >= 0, sc, -1e30)
        a = jax.nn.softmax(sc, axis=-1)
        attn = a[:, :, 0] - lam * a[:, :, 1]
        outs.append(jnp.einsum('bhqk,bkhe->bqhe', attn.astype(v.dtype), vb))
    o = jnp.concatenate(outs, axis=1)
    o = rmsnorm(o, norm_w) * (1.0 - lam_init)
    return o.reshape(b_, s_, h_ * 2 * d)


def mlstm_mixer(q, k, v, o_raw, i_raw, f_raw, i_bias, f_bias, norm_w):
    b_, s_, _ = q.shape
    dtp = q.dtype
    f32 = jnp.float32
    H, d, L = MLSTM_HEADS, MLSTM_HEAD_DIM, MLSTM_CHUNK
    nc = s_ // L
    qc = q.reshape(b_, nc, L, H, d)
    kc = k.reshape(b_, nc, L, H, d) * (d ** -0.5)
    vc = v.reshape(b_, nc, L, H, d)
    log_i = (i_raw.astype(f32) + i_bias.astype(f32)).reshape(b_, nc, L, H).transpose(0, 3, 1, 2)
    log_f = jax.nn.log_sigmoid(f_raw.astype(f32) + f_bias.astype(f32)).reshape(b_, nc, L, H).transpose(0, 3, 1, 2)
    bcum = jnp.cumsum(log_f, axis=-1)
    mask = jnp.tril(jnp.ones((L, L), dtype=bool))
    log_d = jnp.where(mask, bcum[..., :, None] - bcum[..., None, :] + log_i[..., None, :], -jnp.inf)
    m_intra = jnp.max(log_d, axis=-1)
    g = bcum[..., -1]
    w_end = g[..., None] - bcum + log_i
    m_loc = jnp.max(w_end, axis=-1)
    e_end = jnp.exp(w_end - m_loc[..., None]).astype(dtp)
    c_loc = jnp.einsum('bhcs,bcshe,bcshd->bched', e_end, vc, kc)
    n_loc = jnp.einsum('bhcs,bcshd->bchd', e_end, kc)

    def step(carry, inp):
        cm, nm, mm = carry
        cl, nl, ml, gl = inp
        m_new = jnp.maximum(gl + mm, ml)
        a_prev = jnp.exp(gl + mm - m_new).astype(cm.dtype)
        a_loc = jnp.exp(ml - m_new).astype(cm.dtype)
        c_new = cm * a_prev[..., None, None] + cl * a_loc[..., None, None]
        n_new = nm * a_prev[..., None] + nl * a_loc[..., None]
        return (c_new, n_new, m_new), (cm, nm, mm)

    init = (jnp.zeros_like(c_loc[:, 0]), jnp.zeros_like(n_loc[:, 0]), jnp.zeros((b_, H), f32))
    _, (c_prev, n_prev, m_prev) = lax.scan(
        step, init, (jnp.moveaxis(c_loc, 1, 0), jnp.moveaxis(n_loc, 1, 0),
                     jnp.moveaxis(m_loc, 2, 0), jnp.moveaxis(g, 2, 0)))
    c_prev = jnp.moveaxis(c_prev, 0, 1)
    n_prev = jnp.moveaxis(n_prev, 0, 1)
    m_prev = jnp.moveaxis(m_prev, 0, 2)
    inter_log = bcum + m_prev[..., None]
    m_t = jnp.maximum(inter_log, m_intra)
    w_inter = jnp.exp(inter_log - m_t)
    w_intra = jnp.exp(log_d - m_t[..., None])
    a_mat = jnp.einsum('bclhd,bcshd->bhcls', qc, kc) * w_intra.astype(dtp)
    wi = w_inter.transpose(0, 2, 3, 1)
    num = (jnp.einsum('bhcls,bcshe->bclhe', a_mat, vc)
           + wi[..., None].astype(dtp) * jnp.einsum('bclhd,bched->bclhe', qc, c_prev))
    den = jnp.sum(a_mat.astype(f32), axis=-1) + w_inter * jnp.einsum('bclhd,bchd->bhcl', qc, n_prev).astype(f32)
    denom = jnp.maximum(jnp.abs(den), jnp.exp(-m_t))
    h_t = (num / denom.transpose(0, 2, 3, 1)[..., None].astype(dtp)).reshape(b_, s_, H, d)
    h_t = rmsnorm(h_t, norm_w.reshape(H, d))
    out = jax.nn.sigmoid(o_raw).reshape(b_, s_, H, d) * h_t
    return out.reshape(b_, s_, MLSTM_WIDTH)


def setup_inputs(seed: int = 0) -> dict:
    key = jax.random.key(seed)
    ks = jax.random.split(key, 32)
    f32 = jnp.float32

    def nrm(k, shape, scale):
        return jax.random.normal(k, shape, f32) * scale

    x = nrm(ks[0], (BATCH, SEQ, D_MODEL), 1.0)
    p = nrm(ks[1], (DEPTH, BATCH, SEQ, PLE_DIM), 1.0)
    norm1_w = 1.0 + nrm(ks[2], (DEPTH, D_MODEL), 0.02)
    w_in = nrm(ks[3], (DEPTH, D_MODEL, IN_COLS), D_MODEL ** -0.5)
    ssd_conv_w = nrm(ks[4], (DEPTH, SSD_CONV, SSD_CONV_CH), SSD_CONV ** -0.5)
    ssd_conv_b = nrm(ks[5], (DEPTH, SSD_CONV_CH), 0.02)
    dt0 = jnp.exp(jax.random.uniform(ks[6], (DEPTH, SSD_HEADS), f32, math.log(1e-3), math.log(1e-1)))
    ssd_dt_bias = dt0 + jnp.log(-jnp.expm1(-dt0))
    ssd_a_log = jnp.log(jax.random.uniform(ks[7], (DEPTH, SSD_HEADS), f32, 1.0, 16.0))
    ssd_d = 1.0 + nrm(ks[8], (DEPTH, SSD_HEADS), 0.02)
    ssd_norm_w = 1.0 + nrm(ks[9], (DEPTH, SSD_WIDTH), 0.02)
    diff_lq1 = nrm(ks[10], (DEPTH, DIFF_QK_DIM), 0.1)
    diff_lk1 = nrm(ks[11], (DEPTH, DIFF_QK_DIM), 0.1)
    diff_lq2 = nrm(ks[12], (DEPTH, DIFF_QK_DIM), 0.1)
    diff_lk2 = nrm(ks[13], (DEPTH, DIFF_QK_DIM), 0.1)
    diff_norm_w = 1.0 + nrm(ks[14], (DEPTH, DIFF_V_DIM), 0.02)
    rel_bias = nrm(ks[15], (REL_BUCKETS, DIFF_HEADS), 0.5)
    mlstm_i_bias = nrm(ks[16], (DEPTH, MLSTM_HEADS), 0.1)
    mlstm_f_bias = jnp.linspace(3.0, 6.0, MLSTM_HEADS, dtype=f32)[None, :] + nrm(ks[17], (DEPTH, MLSTM_HEADS), 0.1)
    mlstm_norm_w = 1.0 + nrm(ks[18], (DEPTH, MLSTM_WIDTH), 0.02)
    w_out = nrm(ks[19], (DEPTH, MIX_WIDTH, D_MODEL), MIX_WIDTH ** -0.5)
    norm2_w = 1.0 + nrm(ks[20], (DEPTH, D_MODEL), 0.02)
    w_ffn_gate = nrm(ks[21], (DEPTH, D_MODEL, FFN_HIDDEN), D_MODEL ** -0.5)
    w_ffn_up = nrm(ks[22], (DEPTH, D_MODEL, FFN_HIDDEN), D_MODEL ** -0.5)
    w_ffn_down = nrm(ks[23], (DEPTH, FFN_HIDDEN, D_MODEL), FFN_HIDDEN ** -0.5)
    ple_gate_w = nrm(ks[24], (DEPTH, D_MODEL, D_MODEL), D_MODEL ** -0.5)
    ple_proj_w = nrm(ks[25], (DEPTH, PLE_DIM, D_MODEL), PLE_DIM ** -0.5)
    final_norm_w = 1.0 + nrm(ks[26], (D_MODEL,), 0.02)
    return {"x": x, "p": p, "norm1_w": norm1_w, "w_in": w_in,
            "ssd_conv_w": ssd_conv_w, "ssd_conv_b": ssd_conv_b, "ssd_dt_bias": ssd_dt_bias,
            "ssd_a_log": ssd_a_log, "ssd_d": ssd_d, "ssd_norm_w": ssd_norm_w,
            "diff_lq1": diff_lq1, "diff_lk1": diff_lk1, "diff_lq2": diff_lq2, "diff_lk2": diff_lk2,
            "diff_norm_w": diff_norm_w, "rel_bias": rel_bias,
            "mlstm_i_bias": mlstm_i_bias, "mlstm_f_bias": mlstm_f_bias, "mlstm_norm_w": mlstm_norm_w,
            "w_out": w_out, "norm2_w": norm2_w, "w_ffn_gate": w_ffn_gate, "w_ffn_up": w_ffn_up,
            "w_ffn_down": w_ffn_down, "ple_gate_w": ple_gate_w, "ple_proj_w": ple_proj_w,
            "final_norm_w": final_norm_w}


def reference(x, p, norm1_w, w_in, ssd_conv_w, ssd_conv_b, ssd_dt_bias, ssd_a_log, ssd_d, ssd_norm_w,
              diff_lq1, diff_lk1, diff_lq2, diff_lk2, diff_norm_w, rel_bias,
              mlstm_i_bias, mlstm_f_bias, mlstm_norm_w, w_out, norm2_w, w_ffn_gate, w_ffn_up,
              w_ffn_down, ple_gate_w, ple_proj_w, final_norm_w):
    b_, s_, _ = x.shape
    f32 = jnp.float32
    h = x
    for i in range(DEPTH):
        u = rmsnorm(h, norm1_w[i])
        proj = u @ w_in[i]
        z, xbc, dt_raw, dq, dk, dv, mq, mk, mv, mo, mi, mf = split_cols(proj, IN_SPLITS)
        y_ssd = ssd_mixer(z, xbc, dt_raw, ssd_conv_w[i], ssd_conv_b[i], ssd_dt_bias[i],
                          ssd_a_log[i], ssd_d[i], ssd_norm_w[i])
        lam_init = 0.8 - 0.6 * math.exp(-0.3 * i)
        lam = (jnp.exp(jnp.sum(diff_lq1[i].astype(f32) * diff_lk1[i].astype(f32)))
               - jnp.exp(jnp.sum(diff_lq2[i].astype(f32) * diff_lk2[i].astype(f32))) + lam_init)
        y_diff = diff_attention(dq.reshape(b_, s_, DIFF_HEADS, 2, DIFF_QK_DIM),
                                dk.reshape(b_, s_, DIFF_HEADS, 2, DIFF_QK_DIM),
                                dv.reshape(b_, s_, DIFF_HEADS, DIFF_V_DIM),
                                lam, lam_init, rel_bias, diff_norm_w[i])
        y_mlstm = mlstm_mixer(mq, mk, mv, mo, mi, mf, mlstm_i_bias[i], mlstm_f_bias[i], mlstm_norm_w[i])
        h = h + jnp.concatenate([y_ssd, y_diff, y_mlstm], axis=-1) @ w_out[i]
        u = rmsnorm(h, norm2_w[i])
        h = h + (jax.nn.silu(u @ w_ffn_gate[i]) * (u @ w_ffn_up[i])) @ w_ffn_down[i]
        h = h + jax.nn.sigmoid(h @ ple_gate_w[i]) * (p[i] @ ple_proj_w[i])
    return rmsnorm(h, final_norm_w)
```

```python
import math
import numpy as np
import concourse.bass as bass
import concourse.mybir as mybir
from concourse.bass_utils import run_bass_kernel_spmd
from contextlib import ExitStack

F32 = mybir.dt.float32
BF16 = mybir.dt.bfloat16
AF = mybir.ActivationFunctionType
ALU = mybir.AluOpType

N_DMA_SEMS = 24
NEG = -1.0e30
EPS = 1e-6
IN_COLS = 3344
FFN_H = 2816
O_Z, O_XBC, O_DT, O_DQ, O_DK, O_DV, O_MQ, O_MK, O_MV, O_MO, O_MI = (
    0, 512, 1536, 1544, 1800, 2056, 2312, 2568, 2824, 3080, 3336)


class Buf:
    __slots__ = ("name", "writer", "readers", "excl")

    def __init__(self, name="", excl=False):
        self.name = name
        self.writer = None
        self.readers = []
        self.excl = excl


class Op:
    __slots__ = ("eng", "fn", "deps", "is_dma", "marked", "token", "idx")

    def __init__(self, eng, fn, is_dma):
        self.eng = eng
        self.fn = fn
        self.deps = []
        self.is_dma = is_dma
        self.marked = is_dma
        self.token = None
        self.idx = 0


class Sched:
    ENGS = ("pe", "act", "dve", "pool", "sp")

    def __init__(self, nc):
        self.nc = nc
        self.eng_ops = {e: [] for e in self.ENGS}
        self.dma_ops = []
        self.n_ops = 0

    def op(self, eng, fn, reads=(), writes=(), dma=False):
        o = Op(eng, fn, dma)
        deps = {}
        for b in reads:
            if b.writer is not None:
                deps[id(b.writer)] = b.writer
            if b.excl:
                for r in b.readers:
                    if r.eng != eng:
                        deps[id(r)] = r
        for b in writes:
            if b.writer is not None:
                deps[id(b.writer)] = b.writer
            for r in b.readers:
                deps[id(r)] = r
        if dma:
            if len(self.dma_ops) >= N_DMA_SEMS:
                p = self.dma_ops[len(self.dma_ops) - N_DMA_SEMS]
                deps[id(p)] = p
            o.idx = len(self.dma_ops)
            self.dma_ops.append(o)
        for d in deps.values():
            if d.eng == "pe" and eng == "pe" and not d.is_dma and not dma:
                continue
            d.marked = True
            o.deps.append(d)
        for b in writes:
            b.writer = o
            b.readers = []
        for b in reads:
            if b.writer is not o:
                b.readers.append(o)
        self.eng_ops[eng].append(o)
        self.n_ops += 1
        return o

    def emit(self, final_waits=()):
        nc = self.nc
        with ExitStack() as es:
            esem = {e: es.enter_context(nc.semaphore("s_" + e)) for e in self.ENGS}
            dsem = [es.enter_context(nc.semaphore("s_dma%d" % i)) for i in range(N_DMA_SEMS)]
            cnt = {e: 0 for e in self.ENGS}
            for o in self.dma_ops:
                o.token = (dsem[o.idx % N_DMA_SEMS], 16 * (o.idx // N_DMA_SEMS + 1), 16)
            for e in self.ENGS:
                for o in self.eng_ops[e]:
                    if not o.is_dma and o.marked:
                        cnt[e] += 1
                        o.token = (esem[e], cnt[e], 1)
            block = es.enter_context(nc.Block())

            def run(e, h):
                seen = {}
                for o in self.eng_ops[e]:
                    waits = {}
                    for d in o.deps:
                        s, v, _ = d.token
                        k = id(s)
                        if k not in waits or waits[k][1] < v:
                            waits[k] = (s, v)
                    for k, (s, v) in waits.items():
                        if seen.get(k, 0) >= v:
                            continue
                        h.wait_ge(s, v)
                        seen[k] = v
                    ins = o.fn(h)
                    if o.token is not None:
                        ins.then_inc(o.token[0], o.token[2])
                if e == "sp":
                    waits = {}
                    for d in final_waits:
                        s, v, _ = d.token
                        k = id(s)
                        if k not in waits or waits[k][1] < v:
                            waits[k] = (s, v)
                    for k, (s, v) in waits.items():
                        h.wait_ge(s, v)

            @block.tensor
            def _(h):
                run("pe", h)

            @block.scalar
            def _(h):
                run("act", h)

            @block.vector
            def _(h):
                run("dve", h)

            @block.gpsimd
            def _(h):
                run("pool", h)

            @block.sync
            def _(h):
                run("sp", h)


def build(S, NSEQ, NL, wplan=None, dbg=False):
    NT = S // 128
    NG = S // 512
    nc = bass.Bass("TRN2", target_bir_lowering=False)
    sch = Sched(nc)
    record_plan = wplan is None
    plan_out = []

    def din(name, shape):
        return nc.dram_tensor(name, list(shape), F32, kind="ExternalInput")

    x_d = din("x", [NSEQ, S, 1024])
    p_d = din("p", [NL, NSEQ, S, 256])
    win_d = din("w_in", [NL, 1024, IN_COLS])
    wout_d = din("w_out", [NL, 1024, 1024])
    wg_d = din("w_ffn_gate", [NL, 1024, FFN_H])
    wu_d = din("w_ffn_up", [NL, 1024, FFN_H])
    wd_d = din("w_ffn_down", [NL, FFN_H, 1024])
    wpg_d = din("ple_gate_w", [NL, 1024, 1024])
    wpp_d = din("ple_proj_w", [NL, 256, 1024])
    cst_d = din("consts", [128, 6 * 128])
    bt_d = din("bt", [128, 4 * 256])
    NV = 8 + 8 + 8 + 8 + 32 + 4 + 2 + 2
    NR = 8 + 8 + 8 + 4 + 4 + 128 + 4
    vec_d = din("vecs", [NL, 128, NV])
    row_d = din("rows", [NL, 128, NR])
    out_d = nc.dram_tensor("out", [NSEQ, S, 1024], F32, kind="ExternalOutput")
    dbg_d = nc.dram_tensor("dbg", [128, 8, S], F32, kind="ExternalOutput") if dbg else None

    hT = nc.alloc_sbuf_tensor("hT", [128, 8, S], F32)
    hB = [[Buf() for _ in range(NG)] for _ in range(8)]
    uT = nc.alloc_sbuf_tensor("uT", [128, 8, S], BF16)
    uB = [[Buf() for _ in range(NG)] for _ in range(8)]
    NU = 61
    NF = 16
    arena = nc.alloc_sbuf_tensor("arena", [128, NU, 512], BF16)
    aB = [Buf("a%d" % i) for i in range(NU)]
    farena = nc.alloc_sbuf_tensor("farena", [128, NF, 256], F32)
    fB = [Buf("f%d" % i) for i in range(NF)]
    NW = 2
    wts = [nc.alloc_sbuf_tensor("wt%d" % i, [128, 8, 512], BF16) for i in range(NW)]
    wB = [Buf() for _ in range(NW)]
    wstg = [nc.alloc_sbuf_tensor("wstg%d" % i, [128, 1, 512], F32) for i in range(2)]
    wsB = [Buf() for _ in range(2)]
    cst = nc.alloc_sbuf_tensor("cst", [128, 6 * 128], F32)
    cB = Buf()
    cbf = nc.alloc_sbuf_tensor("cbf", [128, 3 * 128 + 260], BF16)
    cbB = Buf()
    vec = nc.alloc_sbuf_tensor("vec", [128, NL, NV], F32)
    row = nc.alloc_sbuf_tensor("row", [128, NL, NR], F32)
    vrB = Buf()
    small = nc.alloc_sbuf_tensor("small", [128, 64], F32)
    smB = Buf()
    smP = [[Buf() for _ in range(3)] for _ in range(2)]
    gates = nc.alloc_sbuf_tensor("gates", [128, 3, NT, 8], F32)
    gB = [Buf() for _ in range(4)]
    ssdS = nc.alloc_sbuf_tensor("ssdS", [128, 8, 64], F32)
    ssdSb = nc.alloc_sbuf_tensor("ssdSb", [128, 8, 64], BF16)
    mlS = nc.alloc_sbuf_tensor("mlS", [128, 2, 2, 64], F32)
    mlSb = nc.alloc_sbuf_tensor("mlSb", [128, 2, 2, 64], BF16)
    stB = [Buf() for _ in range(4)]
    banks = [nc.alloc_psum_tensor("bank%d" % i, [128, 512], F32) for i in range(8)]
    bkB = [Buf(excl=True) for _ in range(8)]
    st = {"ps": 0, "w": 0, "wq": 0, "ws": 0}

    ident_f = cst[:, 0:128]
    tri_f = cst[:, 128:256]
    mask_f = cst[:, 256:384]
    ones_f = cst[:, 384:512]
    sup_f = cst[:, 640:768]
    ident_b = cbf[:, 0:128]
    ones_b = cbf[:, 128:256]
    blk_b = cbf[:, 256:384]
    zero_b = cbf[:, 384:512]

    def ps():
        i = st["ps"] % 6
        st["ps"] += 1
        return banks[i], bkB[i]

    def ps_held(j):
        return banks[6 + j], bkB[6 + j]

    def abf(u, n=1):
        return arena[:, u:u + n, :].rearrange("p a b -> p (a b)")

    def ff(u, n):
        return farena[:, u:u + n, :].rearrange("p a b -> p (a b)")

    def fb(u, n=1):
        return fB[u:u + n]

    F_RT, F_TMP, F_X = 0, 4, 12

    def ab(u, n=1):
        return aB[u:u + n]

    def ACT(out, in_, func, reads, writes, bias=None, scale=None):
        kw = {}
        if bias is not None:
            kw["bias"] = bias
        if scale is not None:
            kw["scale"] = scale
        sch.op("act", lambda h: h.activation(out=out, in_=in_, func=func, **kw), reads, writes)

    def TT(eng, out, in0, in1, op, reads, writes):
        sch.op(eng, lambda h: h.tensor_tensor(out=out, in0=in0, in1=in1, op=op), reads, writes)

    def TS(eng, out, in0, s1, op0, reads, writes, s2=None, op1=None):
        if op1 is None:
            sch.op(eng, lambda h: h.tensor_scalar(out=out, in0=in0, scalar1=s1, scalar2=None, op0=op0), reads, writes)
        else:
            sch.op(eng, lambda h: h.tensor_scalar(out=out, in0=in0, scalar1=s1, scalar2=s2, op0=op0, op1=op1), reads, writes)

    def STT(out, in0, scalar, in1, op0, op1, reads, writes):
        sch.op("dve", lambda h: h.scalar_tensor_tensor(out=out, in0=in0, scalar=scalar, in1=in1, op0=op0, op1=op1), reads, writes)

    def CP(eng, out, in_, reads, writes):
        if eng == "act":
            sch.op("act", lambda h: h.copy(out=out, in_=in_), reads, writes)
        else:
            sch.op(eng, lambda h: h.tensor_copy(out=out, in_=in_), reads, writes)

    def MSET(eng, ap, val, writes):
        sch.op(eng, lambda h: h.memset(ap, val), (), writes)

    def RECIP(out, in_, reads, writes):
        sch.op("dve", lambda h: h.reciprocal(out=out, in_=in_), reads, writes)

    def MMG(mms, reads, writes):
        def fn(h):
            ins = None
            for m in mms:
                kw = {}
                if m.get("tp") is not None:
                    kw["tile_position"] = m["tp"]
                if m.get("sgc"):
                    kw["skip_group_check"] = True
                ins = h.matmul(m["out"], lhsT=m["lhsT"], rhs=m["rhs"], start=m["start"], stop=m["stop"], **kw)
            return ins
        sch.op("pe", fn, reads, writes)

    def TRG(trs, reads, writes):
        def fn(h):
            ins = None
            for (o, i, idn) in trs:
                ins = h.transpose(out=o, in_=i, identity=idn, tile_position=(0, 0))
            return ins
        sch.op("pe", fn, reads, writes)

    def DMA(eng, out, in_, reads, writes):
        return sch.op(eng, lambda h: h.dma_start(out=out, in_=in_), reads, writes, dma=True)

    wspecs = wplan if wplan is not None else []

    def w_issue(k):
        (name, l, r0, nr, c0, ncw) = wspecs[k]
        src_t = {"w_in": win_d, "w_out": wout_d, "wg": wg_d, "wu": wu_d, "wd": wd_d, "wpg": wpg_d, "wpp": wpp_d}[name]
        kch = nr // 128
        src = src_t[l, r0:r0 + nr, c0:c0 + ncw].rearrange("(k p) n -> p k n", p=128)
        i = k % NW
        for k0 in range(0, kch, 1):
            nk = 1
            j = st["ws"] % 2
            st["ws"] += 1
            DMA("sp", wstg[j][:, 0:nk, 0:ncw], src[:, k0:k0 + nk, :], (), [wsB[j]])
            CP("pool", wts[i][:, k0:k0 + nk, 0:ncw], wstg[j][:, 0:nk, 0:ncw], [wsB[j]], [wB[i]])

    def W(name, l, r0, nr, c0, ncw, hold=0):
        spec = (name, l, r0, nr, c0, ncw)
        k = st["w"]
        st["w"] += 1
        if record_plan:
            plan_out.append(spec)
            return wts[0], wB[0]
        assert wspecs[k] == spec, (k, wspecs[k], spec)
        while st["wq"] < min(len(wspecs), k + NW - hold):
            w_issue(st["wq"])
            st["wq"] += 1
        return wts[k % NW], wB[k % NW]

    def prologue():
        DMA("sp", cst[:], cst_d[:], (), [cB])
        DMA("sp", vec[:], vec_d.ap().rearrange("l p n -> p l n"), (), [vrB])
        DMA("sp", row[:], row_d.ap().rearrange("l p n -> p l n"), (), [vrB])
        CP("dve", cbf[:, 0:128], cst[:, 0:128], [cB], [cbB])
        CP("dve", cbf[:, 128:256], cst[:, 384:512], [cB], [cbB])
        CP("dve", cbf[:, 256:384], cst[:, 512:640], [cB], [cbB])
        MSET("dve", cbf[:, 384:644], 0.0, [cbB])
        MSET("dve", small[:, 0:1], EPS, [smB])
        MSET("dve", small[:, 1:2], 1.0, [smB])

    def vcol(l, off, n=1):
        return vec[:, l, off:off + n]

    V_N1, V_N2, V_NF, V_CB, V_CW, V_SN, V_DN, V_MN = 0, 8, 16, 24, 32, 64, 68, 70
    R_DTB, R_ALOG, R_DSK, R_IB, R_FB, R_LAM, R_FAR, R_DSKX = 0, 8, 16, 24, 28, 32, 160, 164

    SCR = 48

    def rstd_from_ss(ssb, ssB, rt, rtB, n, divisor):
        ACT(rt, ssb, AF.Sqrt, [ssB, smB], rtB, bias=small[:, 0:1], scale=1.0 / divisor)
        RECIP(rt, rt, rtB, rtB)

    def rmsnorm_h(wc, dst, dstB, dst_f32=False):
        for g in range(NG):
            sl = slice(g * 512, (g + 1) * 512)
            ssb, ssB = ps()
            for c in range(8):
                u = SCR + (c % 2)
                ACT(abf(u), hT[:, c, sl], AF.Square, [hB[c][g]], ab(u))
                MMG([dict(out=ssb[:], lhsT=ones_b, rhs=abf(u), start=(c == 0), stop=(c == 7))], ab(u) + [cbB], [ssB])
            ru = F_RT + 2 * (g % 2)
            rt = ff(ru, 2)
            rstd_from_ss(ssb[:], ssB, rt, fb(ru, 2), 512, 1024.0)
            for c in range(8):
                STT(dst(c, g), hT[:, c, sl], wc[:, c:c + 1], rt, ALU.mult, ALU.mult,
                    [hB[c][g], vrB] + fb(ru, 2), dstB(c, g))

    def u_ap(c, g):
        return uT[:, c, g * 512:(g + 1) * 512]

    def u_b(c, g):
        return [uB[c][g]]

    def dense_fm(wt, wbuf, kch, col0, rhs_ap, rhs_bufs, g, ncol=128):
        pb, pbB = ps()
        mms = [dict(out=pb[0:ncol, :], lhsT=wt[:, k, col0:col0 + ncol], rhs=rhs_ap(k, g),
                    start=(k == 0), stop=(k == kch - 1)) for k in range(kch)]
        rb = [wbuf]
        for k in range(kch):
            rb += rhs_bufs(k, g)
        MMG(mms, rb, [pbB])
        return pb, pbB

    def add_to_h(c, g, pb, pbB):
        sl = slice(g * 512, (g + 1) * 512)
        TT("dve", hT[:, c, sl], hT[:, c, sl], pb[:], ALU.add, [hB[c][g], pbB], [hB[c][g]])

    def wout_partial(l, k0, nk, src_ap, src_bufs):
        for half in range(2):
            wt, wbuf = W("w_out", l, k0 * 128, nk * 128, half * 512, 512)
            for cc in range(4):
                c = half * 4 + cc
                for g in range(NG):
                    pb, pbB = dense_fm(wt, wbuf, nk, cc * 128, src_ap, src_bufs, g)
                    add_to_h(c, g, pb, pbB)

    def post_norm(u0, nch, mode, wcol, gate=None):
        for g in range(NG):
            for c in range(nch):
                u = u0 + c * NG + g
                if mode == "all" and c > 0:
                    pass
                else:
                    ssb, ssB = ps()
                if mode == "all":
                    squ = SCR + (c % 2)
                    ACT(abf(squ), abf(u), AF.Square, ab(u), ab(squ))
                    MMG([dict(out=ssb[:], lhsT=ones_b, rhs=abf(squ), start=(c == 0), stop=(c == nch - 1))],
                        ab(squ) + [cbB], [ssB])
                    if c < nch - 1:
                        continue
                    ru = F_RT + 2 * (g % 2)
                    rstd_from_ss(ssb[:], ssB, ff(ru, 2), fb(ru, 2), 512, nch * 128.0)
                    for c2 in range(nch):
                        u2 = u0 + c2 * NG + g
                        STT(abf(u2), abf(u2), wcol[:, c2:c2 + 1], ff(ru, 2), ALU.mult, ALU.mult,
                            ab(u2) + fb(ru, 2) + [vrB, smB], ab(u2))
                else:
                    squ = SCR + (c % 2)
                    ACT(abf(squ), abf(u), AF.Square, ab(u), ab(squ))
                    MMG([dict(out=ssb[:], lhsT=blk_b, rhs=abf(squ), start=True, stop=True)], ab(squ) + [cbB], [ssB])
                    ru = F_RT + 2 * (c % 2)
                    rstd_from_ss(ssb[:], ssB, ff(ru, 2), fb(ru, 2), 512, 64.0)
                    STT(abf(u), abf(u), wcol[:, c:c + 1], ff(ru, 2), ALU.mult, ALU.mult,
                        ab(u) + fb(ru, 2) + [vrB, smB], ab(u))
                    if gate is not None:
                        gate(c, g, u)

    U_D, U_E, U_MT, U_CP, U_XB, U_XP, U_XD, U_XPP = (
        SCR + 2, SCR + 3, SCR + 4, SCR + 5, SCR + 6, SCR + 8, SCR + 9, SCR + 10)

    def decay_unit(par, adt_ap, adtB, nh_tot, hsel):
        h0 = hsel
        ru = (F_X, F_TMP)[par]
        rhs = ff(ru, 2).rearrange("p (h l) -> p h l", h=4)
        TT("pool", rhs, tri_f.unsqueeze(1).to_broadcast([128, 4, 128]),
           adt_ap[:, h0:h0 + 4].unsqueeze(2).to_broadcast([128, 4, 128]), ALU.mult, [cB] + adtB, fb(ru, 2))
        Rb, RB = ps()
        MMG([dict(out=Rb[:], lhsT=ones_f, rhs=ff(ru, 2), start=True, stop=True)], fb(ru, 2) + [cB], [RB])
        Tb, TB = ps()
        MMG([dict(out=Tb[:], lhsT=sup_f, rhs=ff(ru, 2), start=True, stop=True)], fb(ru, 2) + [cB], [TB])
        so = 8 + 16 * par
        tu = (F_X + 2, F_TMP + 2)[par]
        T1 = ff(tu, 2).rearrange("p (h l) -> p h l", h=4)
        R3 = Rb[:].rearrange("p (h l) -> p h l", h=4)
        TT("dve", T1, Tb[:].rearrange("p (h l) -> p h l", h=4), mask_f.unsqueeze(1).to_broadcast([128, 4, 128]),
           ALU.add, [TB, cB], fb(tu, 2))
        du = (U_D, SCR + 0)[par]
        D = abf(du).rearrange("p (h l) -> p h l", h=4)
        ACT(D, T1, AF.Exp, fb(tu, 2), ab(du))
        dlast = small[:, so + 4:so + 8]
        ACT(dlast, T1[:, :, 127], AF.Exp, fb(tu, 2), [smP[par][1]])
        eu = (U_E, SCR + 1)[par]
        E = abf(eu).rearrange("p (h l) -> p h l", h=4)
        ACT(E, R3, AF.Exp, [RB], ab(eu))
        elast = small[:, so + 8:so + 12]
        ACT(elast, R3[:, :, 127], AF.Exp, [RB], [smP[par][2]])
        return D, ab(du), E, ab(eu), dlast, elast

    def ssd_phase(l, q):
        YG, XBC = 0, 16
        ST = SCR
        for half in range(2):
            wt, wbuf = W("w_in", l, 0, 1024, O_XBC + half * 512, 512)
            for cc in range(4):
                j = half * 4 + cc
                sg = ST + 5 * (j % 2)
                stage = arena[:, sg:sg + 5, :].rearrange("p a b -> p (a b)")
                du = SCR + 10 + (j % 2)
                dg = abf(du).rearrange("p (k c) -> p k c", k=4)
                for k in range(4):
                    TS("dve", dg[:, k, :], ident_f, vcol(l, V_CW + k * 8 + j), ALU.mult, [cB, vrB], ab(du))
                MSET("dve", stage[:, 0:3], 0.0, ab(sg))
                for g in range(NG):
                    pb, pbB = dense_fm(wt, wbuf, 8, cc * 128, lambda k, g_: u_ap(k, g_), lambda k, g_: u_b(k, g_), g)
                    CP("act", stage[:, 3 + g * 512:3 + (g + 1) * 512], pb[:], [pbB], ab(sg + g) + ab(sg + g + 1))
                for g in range(NG):
                    cb_, cbB_ = ps()
                    mms = [dict(out=cb_[:], lhsT=dg[:, k, :], rhs=stage[:, g * 512 + k:g * 512 + k + 512],
                                start=(k == 0), stop=(k == 3)) for k in range(4)]
                    MMG(mms, ab(du) + ab(sg + g) + ab(sg + g + 1) + (ab(sg + g - 1) if g > 0 else []), [cbB_])
                    u = XBC + j * NG + g
                    ACT(abf(u), cb_[:], AF.Silu, [cbB_, vrB], ab(u), bias=vcol(l, V_CB + j))
        wt, wbuf = W("w_in", l, 0, 1024, O_DT, 8)
        pb, pbB = ps()
        for t in range(NT):
            mms = [dict(out=pb[:, t * 8:(t + 1) * 8], lhsT=uT[:, k, t * 128:(t + 1) * 128], rhs=wt[:, k, 0:8],
                        start=(k == 0), stop=(k == 7)) for k in range(8)]
            MMG(mms, [wbuf] + [uB[k][t // 4] for k in range(8)], [pbB])
        dt3 = gates[:, 0, :, :]
        adt3 = gates[:, 1, :, :]
        TT("dve", dt3, pb[:, 0:NT * 8].rearrange("p (t h) -> p t h", h=8),
           row[:, l, R_DTB:R_DTB + 8].unsqueeze(1).to_broadcast([128, NT, 8]), ALU.add, [pbB, vrB], [gB[0]])
        ACT(dt3, dt3, AF.Exp, [gB[0]], [gB[0]])
        ACT(dt3, dt3, AF.Ln, [gB[0], smB], [gB[0]], bias=small[:, 1:2])
        ACT(small[:, 40:48], row[:, l, R_ALOG:R_ALOG + 8], AF.Exp, [vrB], [smB])
        STT(adt3, dt3, -1.0, small[:, 40:48].unsqueeze(1).to_broadcast([128, NT, 8]), ALU.mult, ALU.mult,
            [gB[0], smB], [gB[1]])
        MSET("dve", ssdS[:], 0.0, [stB[0]])
        MSET("dve", ssdSb[:], 0.0, [stB[1]])
        dskx = row[:, l, R_DSK:R_DSK + 8].unsqueeze(2).to_broadcast([128, 8, 64])
        for c in range(NT):
            g = c // 4
            off = (c % 4) * 128
            par = c % 2

            def xb_ap(j):
                return abf(XBC + j * NG + g)[:, off:off + 128]

            tb, tbB = ps()
            tbv = tb[:].bitcast(BF16)
            TRG([(tbv[:, j * 128:(j + 1) * 128], xb_ap(j), ident_b) for j in range(6)],
                [aB[XBC + j * NG + g] for j in range(6)] + [cbB], [tbB])
            xu = U_XB
            xB_ = abf(xu, 2)[:, 0:768]
            CP("act", xB_, tbv[:, 0:768], [tbB], ab(xu, 2))
            xs3 = xB_[:, 0:512].rearrange("p (h e) -> p h e", h=8)
            xpu, xdu, xppu = U_XP, U_XD, U_XPP
            Xp = abf(xpu).rearrange("p (h e) -> p h e", h=8)
            Xd = abf(xdu).rearrange("p (h e) -> p h e", h=8)
            Xpp = abf(xppu).rearrange("p (h e) -> p h e", h=8)
            TT("dve", Xp, xs3, gates[:, 0, c, :].unsqueeze(2).to_broadcast([128, 8, 64]), ALU.mult,
               ab(xu, 2) + [gB[0]], ab(xpu))
            TT("pool", Xd, xs3, dskx, ALU.mult, ab(xu, 2) + [vrB], ab(xdu))
            gr = []
            for grp in range(2):
                D, DB, E, EB, dlast, elast = decay_unit(grp, gates[:, 1, c, :], [gB[1]], 8, grp * 4)
                Gb, GB = ps()
                MMG([dict(out=Gb[:, 0:128], lhsT=xb_ap(4 + grp), rhs=xb_ap(6 + grp), start=True, stop=True)],
                    [aB[XBC + (4 + grp) * NG + g], aB[XBC + (6 + grp) * NG + g]], [GB])
                mu = (U_MT, SCR + 11)[grp]
                MT = abf(mu).rearrange("p (h l) -> p h l", h=4)
                TT("dve", MT, D, Gb[:, 0:128].unsqueeze(1).to_broadcast([128, 4, 128]), ALU.mult, DB + [GB], ab(mu))
                cu = (U_CP, SCR + 12)[grp]
                CpT = abf(cu).rearrange("p (h l) -> p h l", h=4)
                TT("pool", CpT, E, xb_ap(6 + grp).unsqueeze(1).to_broadcast([128, 4, 128]), ALU.mult,
                   EB + [aB[XBC + (6 + grp) * NG + g]], ab(cu))
                gr.append((MT, mu, CpT, cu, dlast, elast))
            for grp in range(2):
                MT, mu, CpT, cu, dlast, elast = gr[grp]
                Yb, YB = ps()
                Y4 = Yb[:].rearrange("p (t l) -> p t l", t=4)
                mms = []
                for hh in range(4):
                    hd = grp * 4 + hh
                    o = Y4[(hd % 2) * 64:(hd % 2) * 64 + 64, hh // 2, :]
                    mms.append(dict(out=o, lhsT=Xd[:, hd, :], rhs=ident_b, start=True, stop=False))
                    mms.append(dict(out=o, lhsT=Xp[:, hd, :], rhs=MT[:, hh, :], start=False, stop=False))
                    mms.append(dict(out=o, lhsT=ssdSb[:, hd, :], rhs=CpT[:, hh, :], start=False, stop=True))
                MMG(mms, ab(xdu) + ab(xpu) + ab(mu) + ab(cu) + [stB[1], cbB], [YB])
                for jj in range(2):
                    u = YG + (grp * 2 + jj) * NG + g
                    CP("act" if jj else "dve", abf(u)[:, off:off + 128], Y4[:, jj, :], [YB], ab(u))
                TT("dve", Xpp[:, grp * 4:grp * 4 + 4, :], Xp[:, grp * 4:grp * 4 + 4, :],
                   dlast.unsqueeze(2).to_broadcast([128, 4, 64]), ALU.mult, ab(xpu) + [smP[grp][1]], ab(xppu))
                Sb_, SB_ = ps()
                S3 = Sb_[:, 0:256].rearrange("p (h e) -> p h e", h=4)
                mms = [dict(out=S3[:, hh, :], lhsT=xB_[:, 512 + grp * 128:512 + (grp + 1) * 128],
                            rhs=Xpp[:, grp * 4 + hh, :], start=True, stop=True) for hh in range(4)]
                MMG(mms, ab(xu, 2) + ab(xppu), [SB_])
                Sg = ssdS[:, grp * 4:grp * 4 + 4, :]
                TT("pool", Sg, Sg, elast.unsqueeze(2).to_broadcast([128, 4, 64]), ALU.mult, [stB[0], smP[grp][2]], [stB[0]])
                TT("dve", Sg, Sg, S3, ALU.add, [stB[0], SB_], [stB[0]])
                CP("act", ssdSb[:, grp * 4:grp * 4 + 4, :], Sg, [stB[0]], [stB[1]])
        import os
        if os.environ.get("SSD_STEP") == "1":
            return
        wt, wbuf = W("w_in", l, 0, 1024, O_Z, 512)
        for j in range(4):
            for g in range(NG):
                pb, pbB = dense_fm(wt, wbuf, 8, j * 128, lambda k, g_: u_ap(k, g_), lambda k, g_: u_b(k, g_), g)
                su = F_TMP + 2 * ((j * NG + g) % 2)
                ACT(ff(su, 2), pb[:], AF.Silu, [pbB], fb(su, 2))
                u = YG + j * NG + g
                TT("dve", abf(u), abf(u), ff(su, 2), ALU.mult, ab(u) + fb(su, 2), ab(u))
        post_norm(YG, 4, "all", vcol(l, V_SN, 4))
        wout_partial(l, 0, 4, lambda k, g_: abf(YG + k * NG + g_), lambda k, g_: ab(YG + k * NG + g_))

    def diff_phase(l, q):
        OD, QT, KT, VP, BT = 0, 16, 24, 32, 42
        SCALE = 32 ** -0.5
        btile = ff(F_X, 4).rearrange("p (h c) -> p h c", h=4)
        DMA("sp", ff(F_X, 4), bt_d[:], (), fb(F_X, 4))
        lam_init = 0.8 - 0.6 * math.exp(-0.3 * l)
        lr = row[:, l, R_LAM:R_LAM + 128]
        pr = ff(F_RT, 1)
        TT("dve", pr[:, 0:64], lr[:, 0:64], lr[:, 64:128], ALU.mult, [vrB], fb(F_RT))
        sch.op("dve", lambda h: h.tensor_reduce(out=small[:, 48:50], in_=pr[:, 0:64].rearrange("p (a b) -> p a b", a=2),
                                                axis=mybir.AxisListType.X, op=ALU.add), fb(F_RT), [smB])
        ACT(small[:, 48:50], small[:, 48:50], AF.Exp, [smB], [smB])
        TT("dve", small[:, 50:51], small[:, 48:49], small[:, 49:50], ALU.subtract, [smB], [smB])
        TS("dve", small[:, 51:52], small[:, 50:51], -1.0, ALU.mult, [smB], [smB], s2=-lam_init, op1=ALU.add)
        TS("dve", small[:, 52:53], vcol(l, V_DN), 1.0 - lam_init, ALU.mult, [vrB], [smB])
        wt, wbuf = W("w_in", l, 0, 1024, O_DQ, 512)
        for j in range(4):
            for g in range(NG):
                pb, pbB = dense_fm(wt, wbuf, 8, j * 128, lambda k, g_: u_ap(k, g_), lambda k, g_: u_b(k, g_), g)
                u = (QT if j < 2 else KT) + (j % 2) * NG + g
                CP("act" if (j + g) % 2 else "dve", abf(u), pb[:], [pbB], ab(u))
        wt, wbuf = W("w_in", l, 0, 1024, O_DV, 256)
        VPn = (NT * 260 + 511) // 512
        vp = abf(VP, VPn)[:, 0:NT * 260].rearrange("p (t h e) -> p t h e", t=NT, h=4)
        MSET("pool", vp[:, :, :, 64:65], 1.0, ab(VP, VPn))
        for t in range(NT):
            pb, pbB = ps()
            mms = [dict(out=pb[:, 0:256], lhsT=uT[:, k, t * 128:(t + 1) * 128], rhs=wt[:, k, 0:256],
                        start=(k == 0), stop=(k == 7)) for k in range(8)]
            MMG(mms, [wbuf] + [uB[k][t // 4] for k in range(8)], [pbB])
            CP("act" if t % 2 else "dve", vp[:, t, :, 0:64], pb[:, 0:256].rearrange("p (h e) -> p h e", h=4),
               [pbB], ab(VP, VPn))
        PT0 = SCR + 2
        TM0 = F_TMP
        OT = F_TMP + 4
        ring = {"p": 0, "t": 0}
        for G in range(NG):
            nq = 4
            for hd in range(4):
                accs = []
                for m in range(2):
                    ac, acB = ps_held(m)
                    MMG([dict(out=ac[:, 0:260], lhsT=zero_b, rhs=cbf[:, 384:644], start=True, stop=False, sgc=True)],
                        [cbB], [acB])
                    accs.append((ac, acB))
                ch = hd // 2
                steps = [(i, m) for i in range(4 * G + 4) for m in range(2)]
                pend = {}

                def issue_S(k):
                    i, m = steps[k]
                    pbase = ((hd % 2) * 2 + m) * 32
                    kt_ap = abf(KT + ch * NG + i // 4)[pbase:pbase + 32, (i % 4) * 128:(i % 4) * 128 + 128]
                    qt_ap = abf(QT + ch * NG + G)[pbase:pbase + 32, :]
                    sb, sB = ps()
                    MMG([dict(out=sb[:], lhsT=kt_ap, rhs=qt_ap, start=True, stop=True, tp=(pbase, 0))],
                        [aB[KT + ch * NG + i // 4], aB[QT + ch * NG + G]], [sB])
                    pend[k] = (sb, sB)

                issue_S(0)
                issue_S(1)
                for k in range(len(steps)):
                    i, m = steps[k]
                    sb, sB = pend.pop(k)
                    pu = PT0 + ring["p"] % 4
                    ring["p"] += 1
                    PT = abf(pu)
                    jl0 = max(0, i - 4 * G)
                    far0 = max(jl0, i + 2 - 4 * G)
                    for jl in range(jl0, min(far0, nq)):
                        dlt = 4 * G + jl - i
                        tu = TM0 + (ring["t"] % 4)
                        ring["t"] += 1
                        tmp = ff(tu, 1)[:, 0:128]
                        STT(tmp, sb[:, jl * 128:(jl + 1) * 128], SCALE, btile[:, hd, dlt * 128:(dlt + 1) * 128],
                            ALU.mult, ALU.add, [sB] + fb(F_X, 4), fb(tu))
                        ACT(PT[:, jl * 128:(jl + 1) * 128], tmp, AF.Exp, fb(tu), ab(pu))
                    if far0 < nq:
                        ACT(PT[:, far0 * 128:nq * 128], sb[:, far0 * 128:nq * 128], AF.Exp, [sB, vrB], ab(pu),
                            bias=row[:, l, R_FAR + hd:R_FAR + hd + 1], scale=SCALE)
                    if k + 2 < len(steps):
                        issue_S(k + 2)
                    ac, acB = accs[m]
                    mms = [dict(out=ac[:, jl * 65:(jl + 1) * 65], lhsT=PT[:, jl * 128:(jl + 1) * 128],
                                rhs=vp[:, i, hd, :], start=False, stop=False, sgc=True) for jl in range(jl0, nq)]
                    MMG(mms, ab(pu) + ab(VP, VPn), [acB])
                a0, a0B = accs[0]
                a1, a1B = accs[1]
                A0 = a0[:, 0:260].rearrange("p (j e) -> p j e", j=4)
                A1 = a1[:, 0:260].rearrange("p (j e) -> p j e", j=4)
                so = 24
                RECIP(small[:, so:so + 4], A0[:, :, 64], [a0B], [smB])
                RECIP(small[:, so + 4:so + 8], A1[:, :, 64], [a1B], [smB])
                TS("dve", small[:, so + 4:so + 8], small[:, so + 4:so + 8], small[:, 51:52], ALU.mult, [smB], [smB])
                for jl in range(4):
                    t = 4 * G + jl
                    ou = OT + jl
                    o3 = ff(ou, 1).rearrange("p (h e) -> p h e", h=4)
                    if hd == 0:
                        pass
                    TS("dve", o3[:, hd, :], A0[:, jl, 0:64], small[:, so + jl:so + jl + 1], ALU.mult, [a0B, smB], fb(ou))
                    STT(o3[:, hd, :], A1[:, jl, 0:64], small[:, so + 4 + jl:so + 5 + jl], o3[:, hd, :], ALU.mult, ALU.add,
                        [a1B, smB] + fb(ou), fb(ou))
                    if hd == 3:
                        tb, tbB = ps()
                        TRG([(tb[:, j2 * 128:(j2 + 1) * 128], ff(ou, 1)[:, j2 * 128:(j2 + 1) * 128], ident_f)
                             for j2 in range(2)], fb(ou) + [cB], [tbB])
                        for j2 in range(2):
                            u = OD + j2 * NG + G
                            CP("act", abf(u)[:, jl * 128:(jl + 1) * 128], tb[:, j2 * 128:(j2 + 1) * 128], [tbB], ab(u))
        post_norm(OD, 2, "blk", small[:, 52:53].to_broadcast([128, 2]))
        wout_partial(l, 4, 2, lambda k, g_: abf(OD + k * NG + g_), lambda k, g_: ab(OD + k * NG + g_))

    def mlstm_phase(l, q):
        HM, VT, QT, KT, KTK = 0, 8, 16, 32, 40
        wt, wbuf = W("w_in", l, 0, 1024, O_MQ, 512)
        for j in range(4):
            for g in range(NG):
                pb, pbB = dense_fm(wt, wbuf, 8, j * 128, lambda k, g_: u_ap(k, g_), lambda k, g_: u_b(k, g_), g)
                if j < 2:
                    for hf in range(2):
                        u = QT + (2 * j + hf) * NG + g
                        r0, z0 = hf * 64, (1 - hf) * 64
                        CP("act", abf(u)[r0:r0 + 64, :], pb[r0:r0 + 64, :], [pbB], ab(u))
                        MSET("pool", abf(u)[z0:z0 + 64, :], 0.0, ab(u))
                else:
                    u = KT + (j % 2) * NG + g
                    TS("dve", abf(u), pb[:], 0.125, ALU.mult, [pbB], ab(u))
        wt, wbuf = W("w_in", l, 0, 1024, O_MV, 256)
        vt = abf(VT, NT // 2 if NT >= 2 else 1)[:, 0:NT * 256].rearrange("p (t h e) -> p t h e", t=NT, h=4)
        VTn = max(1, NT // 2)
        for t in range(NT):
            pb, pbB = ps()
            mms = [dict(out=pb[:, 0:256], lhsT=uT[:, k, t * 128:(t + 1) * 128], rhs=wt[:, k, 0:256],
                        start=(k == 0), stop=(k == 7)) for k in range(8)]
            MMG(mms, [wbuf] + [uB[k][t // 4] for k in range(8)], [pbB])
            CP("act" if t % 2 else "dve", vt[:, t, :, :], pb[:, 0:256].rearrange("p (h e) -> p h e", h=4),
               [pbB], ab(VT, VTn))
        wt, wbuf = W("w_in", l, 0, 1024, O_MK, 256)
        ktk = abf(KTK, VTn)[:, 0:NT * 256].rearrange("p (t c) -> p t c", t=NT)
        for t in range(NT):
            pb, pbB = ps()
            mms = [dict(out=pb[:, 0:256], lhsT=uT[:, k, t * 128:(t + 1) * 128], rhs=wt[:, k, 0:256],
                        start=(k == 0), stop=(k == 7)) for k in range(8)]
            MMG(mms, [wbuf] + [uB[k][t // 4] for k in range(8)], [pbB])
            TS("dve", ktk[:, t, :], pb[:, 0:256], 0.125, ALU.mult, [pbB], ab(KTK, VTn))
        wt, wbuf = W("w_in", l, 0, 1024, O_MI, 8)
        pb, pbB = ps()
        for t in range(NT):
            mms = [dict(out=pb[:, t * 8:(t + 1) * 8], lhsT=uT[:, k, t * 128:(t + 1) * 128], rhs=wt[:, k, 0:8],
                        start=(k == 0), stop=(k == 7)) for k in range(8)]
            MMG(mms, [wbuf] + [uB[k][t // 4] for k in range(8)], [pbB])
        g3 = gates[:, 2, :, :]
        TT("dve", g3, pb[:, 0:NT * 8].rearrange("p (t h) -> p t h", h=8),
           row[:, l, R_IB:R_IB + 8].unsqueeze(1).to_broadcast([128, NT, 8]), ALU.add, [pbB, vrB], [gB[2]])
        ACT(g3[:, :, 0:4], g3[:, :, 0:4], AF.Exp, [gB[2]], [gB[2]])
        ACT(g3[:, :, 4:8], g3[:, :, 4:8], AF.Exp, [gB[2]], [gB[2]], scale=-1.0)
        ACT(g3[:, :, 4:8], g3[:, :, 4:8], AF.Ln, [gB[2], smB], [gB[2]], bias=small[:, 1:2])
        TS("dve", g3[:, :, 4:8], g3[:, :, 4:8], -1.0, ALU.mult, [gB[2]], [gB[2]])
        MSET("dve", mlS[:], 0.0, [stB[2]])
        MSET("dve", mlSb[:], 0.0, [stB[3]])
        import os
        for c in range(int(os.environ.get("ML_NCH", NT))):
            g = c // 4
            off = (c % 4) * 128
            par = c % 2

            def q_ap(hd):
                return abf(QT + hd * NG + g)[:, off:off + 128]

            def k_ap(j):
                return abf(KT + j * NG + g)[:, off:off + 128]

            ktok = ktk[:, c, :]
            xpu, xppu = U_XP, U_XPP
            Xp = abf(xpu).rearrange("p (h s e) -> p h s e", h=4, s=2)
            Xpp = abf(xppu).rearrange("p (h s e) -> p h s e", h=4, s=2)
            ei = gates[:, 2, c, 0:4]
            TT("dve", Xp[:, :, 0, :], vt[:, c, :, :], ei.unsqueeze(2).to_broadcast([128, 4, 64]), ALU.mult,
               ab(VT, VTn) + [gB[2]], ab(xpu))
            CP("pool", Xp[:, :, 1, :], ei.unsqueeze(2).to_broadcast([128, 4, 64]), [gB[2]], ab(xpu))
            D, DB, E, EB, dlast, elast = decay_unit(par, gates[:, 2, c, :], [gB[2]], 8, 4)
            Gb, GB = ps()
            G3 = Gb[:].rearrange("p (h l) -> p h l", h=4)
            mms = []
            for hd in range(4):
                b0 = (hd % 2) * 64
                mms.append(dict(out=G3[:, hd, :], lhsT=k_ap(hd // 2), rhs=q_ap(hd), start=True, stop=True))
            MMG(mms, [aB[KT + j * NG + g] for j in range(2)] + [aB[QT + j * NG + g] for j in range(4)], [GB])
            mu = (U_MT, SCR + 11)[par]
            MT = abf(mu).rearrange("p (h l) -> p h l", h=4)
            TT("dve", MT, D, G3, ALU.mult, DB + [GB], ab(mu))
            cu = U_CP + par
            CpT = abf(cu).rearrange("p (h l) -> p h l", h=4)
            for hd in range(4):
                TT("pool", CpT[:, hd, :], E[:, hd, :], q_ap(hd), ALU.mult, EB + [aB[QT + hd * NG + g]], ab(cu))
            Yb, YB = ps()
            Y4 = Yb[:].rearrange("p (t l) -> p t l", t=4)
            mms = []
            for hd in range(4):
                b0 = (hd % 2) * 64
                for s in range(2):
                    o = Y4[b0:b0 + 64, 2 * (hd // 2) + s, :]
                    mms.append(dict(out=o, lhsT=Xp[:, hd, s, :], rhs=MT[:, hd, :], start=True, stop=False))
                    mms.append(dict(out=o, lhsT=mlSb[:, hd // 2, s, :], rhs=CpT[:, hd, :], start=False, stop=True))
            MMG(mms, ab(xpu) + ab(mu) + ab(cu) + [stB[3]], [YB])
            TT("dve", Xpp.rearrange("p h s e -> p h (s e)"), Xp.rearrange("p h s e -> p h (s e)"),
               dlast.unsqueeze(2).to_broadcast([128, 4, 128]), ALU.mult, ab(xpu) + [smP[par][1]], ab(xppu))
            Sb_, SB_ = ps()
            S4 = Sb_[:, 0:256].rearrange("p (a s e) -> p a s e", a=2, s=2)
            mms = []
            for hd in range(4):
                b0 = (hd % 2) * 64
                for s in range(2):
                    mms.append(dict(out=S4[b0:b0 + 64, hd // 2, s, :], lhsT=ktok[:, hd * 64:(hd + 1) * 64],
                                    rhs=Xpp[:, hd, s, :], start=True, stop=True))
            MMG(mms, ab(KTK, VTn) + ab(xppu), [SB_])
            if os.environ.get("ML_DBG"):
                if "dbgS" not in st:
                    st["dbgS"] = nc.alloc_sbuf_tensor("dbgS", [128, 256], F32)
                    st["dbgSB"] = Buf()
                CP("dve", st["dbgS"][:], Sb_[:, 0:256], [SB_], [st["dbgSB"]])
            for hd in range(4):
                b0 = (hd % 2) * 64
                Sg = mlS[b0:b0 + 64, hd // 2, :, :]
                STT(Sg, Sg, elast[b0:b0 + 64, hd:hd + 1], S4[b0:b0 + 64, hd // 2, :, :], ALU.mult, ALU.add,
                    [stB[2], smP[par][2], SB_], [stB[2]])
                CP("act", mlSb[b0:b0 + 64, hd // 2, :, :], Sg, [stB[2]], [stB[3]])
            du = F_TMP + 4
            den = ff(du, 1).rearrange("p (a l) -> p a l", a=2)
            for a in range(2):
                ACT(den[:, a, :], Y4[:, 2 * a + 1, :], AF.Abs, [YB], fb(du))
            TS("dve", ff(du, 1), ff(du, 1), 1.0, ALU.max, fb(du), fb(du))
            RECIP(ff(du, 1), ff(du, 1), fb(du), fb(du))
            for a in range(2):
                u = HM + a * NG + g
                TT("dve", abf(u)[:, off:off + 128], Y4[:, 2 * a, :], den[:, a, :], ALU.mult, [YB] + fb(du), ab(u))
        wt, wbuf = W("w_in", l, 0, 1024, O_MO, 256)

        def gate(cix, g, u):
            pb, pbB = dense_fm(wt, wbuf, 8, cix * 128, lambda k, g_: u_ap(k, g_), lambda k, g_: u_b(k, g_), g)
            su = F_TMP + 2 * ((cix * NG + g) % 2)
            ACT(ff(su, 2), pb[:], AF.Sigmoid, [pbB], fb(su, 2))
            TT("dve", abf(u), abf(u), ff(su, 2), ALU.mult, ab(u) + fb(su, 2), ab(u))

        post_norm(HM, 2, "blk", vcol(l, V_MN, 2), gate=gate)
        wout_partial(l, 6, 2, lambda k, g_: abf(HM + k * NG + g_), lambda k, g_: ab(HM + k * NG + g_))

    def ffn_phase(l, q):
        AT = 0
        rmsnorm_h(vcol(l, V_N2, 8), u_ap, u_b)
        thirds = [(0, 8), (8, 8), (16, 6)]
        for (j0, nj) in thirds:
            for jj0 in range(0, nj, 4):
                njj = min(4, nj - jj0)
                wtg, wbg = W("wg", l, 0, 1024, (j0 + jj0) * 128, njj * 128)
                for jj in range(njj):
                    for g in range(NG):
                        pb, pbB = dense_fm(wtg, wbg, 8, jj * 128, lambda k, g_: u_ap(k, g_), lambda k, g_: u_b(k, g_), g)
                        u = AT + (jj0 + jj) * NG + g
                        ACT(abf(u), pb[:], AF.Silu, [pbB], ab(u))
                wtu, wbu = W("wu", l, 0, 1024, (j0 + jj0) * 128, njj * 128)
                for jj in range(njj):
                    for g in range(NG):
                        pb, pbB = dense_fm(wtu, wbu, 8, jj * 128, lambda k, g_: u_ap(k, g_), lambda k, g_: u_b(k, g_), g)
                        u = AT + (jj0 + jj) * NG + g
                        TT("dve", abf(u), abf(u), pb[:], ALU.mult, ab(u) + [pbB], ab(u))
            for half in range(2):
                wt, wbuf = W("wd", l, j0 * 128, nj * 128, half * 512, 512)
                for cc in range(4):
                    c = half * 4 + cc
                    for g in range(NG):
                        pb, pbB = dense_fm(wt, wbuf, nj, cc * 128, lambda k, g_: abf(AT + k * NG + g_),
                                           lambda k, g_: ab(AT + k * NG + g_), g)
                        add_to_h(c, g, pb, pbB)

    def ple_phase(l, q):
        PTU = 0
        for c in range(8):
            for g in range(NG):
                CP("pool" if (c + g) % 2 else "act", u_ap(c, g), hT[:, c, g * 512:(g + 1) * 512], [hB[c][g]], u_b(c, g))
        import os
        PS_ = int(os.environ.get("PLE_STEP", "9"))
        if PS_ <= 1:
            return
        for t in range(NT):
            su = (F_TMP + 4 + (t % 2)) if PS_ != 23 else (14 + (t % 2))
            ptile = ff(su, 1)
            if PS_ != 22:
                DMA("sp", ptile, p_d[l, q, t * 128:(t + 1) * 128, :], (), fb(su))
            else:
                MSET("dve", ptile, 1.0, fb(su))
            if PS_ in (21, 23):
                continue
            tb, tbB = ps()
            TRG([(tb[:, j * 128:(j + 1) * 128], ptile[:, j * 128:(j + 1) * 128], ident_f) for j in range(2)],
                fb(su) + [cB], [tbB])
            for j in range(2):
                u = PTU + j * NG + t // 4
                CP("act" if j else "dve", abf(u)[:, (t % 4) * 128:(t % 4) * 128 + 128], tb[:, j * 128:(j + 1) * 128],
                   [tbB], ab(u))
        if PS_ <= 2:
            return
        for half in range(2):
            wt1, wb1 = W("wpg", l, 0, 1024, half * 512, 512)
            wt2, wb2 = W("wpp", l, 0, 256, half * 512, 512, hold=1)
            for cc in range(4):
                c = half * 4 + cc
                for g in range(NG):
                    pa, paB = dense_fm(wt1, wb1, 8, cc * 128, lambda k, g_: u_ap(k, g_), lambda k, g_: u_b(k, g_), g)
                    pp, ppB = dense_fm(wt2, wb2, 2, cc * 128, lambda k, g_: abf(PTU + k * NG + g_),
                                       lambda k, g_: ab(PTU + k * NG + g_), g)
                    su = F_TMP + 2 * ((cc * NG + g) % 2)
                    ACT(ff(su, 2), pa[:], AF.Sigmoid, [paB], fb(su, 2))
                    TT("dve", ff(su, 2), ff(su, 2), pp[:], ALU.mult, fb(su, 2) + [ppB], fb(su, 2))
                    sl = slice(g * 512, (g + 1) * 512)
                    TT("dve", hT[:, c, sl], hT[:, c, sl], ff(su, 2), ALU.add, [hB[c][g]] + fb(su, 2), [hB[c][g]])

    def load_x(q):
        for t in range(NT):
            su = F_TMP + 4 * (t % 2)
            xt = ff(su, 4)
            DMA("sp", xt, x_d[q, t * 128:(t + 1) * 128, :], (), fb(su, 4))
            for hf in range(2):
                tb, tbB = ps()
                TRG([(tb[:, j * 128:(j + 1) * 128], xt[:, (hf * 4 + j) * 128:(hf * 4 + j + 1) * 128], ident_f)
                     for j in range(4)], fb(su, 4) + [cB], [tbB])
                CP("act" if hf else "dve", hT[:, hf * 4:hf * 4 + 4, t * 128:(t + 1) * 128],
                   tb[:].rearrange("p (j t) -> p j t", j=4), [tbB], [hB[hf * 4 + j][t // 4] for j in range(4)])

    outs = []

    def store_out(q, l):
        def dst(c, g):
            return hT[:, c, g * 512:(g + 1) * 512]

        def dstB(c, g):
            return [hB[c][g]]

        rmsnorm_h(vcol(l, V_NF, 8), dst, dstB)
        for t in range(NT):
            g = t // 4
            su = F_TMP + 4 * (t % 2)
            ot = ff(su, 4)
            for hf in range(2):
                tb, tbB = ps()
                TRG([(tb[:, j * 128:(j + 1) * 128], hT[:, hf * 4 + j, t * 128:(t + 1) * 128], ident_f) for j in range(4)],
                    [hB[hf * 4 + j][g] for j in range(4)] + [cB], [tbB])
                CP("act" if hf else "dve", ot[:, hf * 512:(hf + 1) * 512], tb[:], [tbB], fb(su, 4))
            outs.append(DMA("sp", out_d[q, t * 128:(t + 1) * 128, :], ot, fb(su, 4), ()))

    def dump_h():
        for c in range(8):
            outs.append(DMA("sp", dbg_d[:, c, :], hT[:, c, :], [hB[c][g] for g in range(NG)], ()))

    stop = int(dbg.split(":")[1]) if isinstance(dbg, str) and dbg.startswith("stop") else 99
    prologue()
    for q in range(NSEQ):
        if stop == 0:
            MSET("dve", hT[:], 1.0, [hB[c][g] for c in range(8) for g in range(NG)])
            dump_h()
            break
        load_x(q)
        if stop <= 1:
            dump_h()
            break
        for l in range(NL):
            rmsnorm_h(vcol(l, V_N1, 8), u_ap, u_b)
            if stop <= 2:
                break
            if dbg != "skipmix" and stop >= 10:
                ssd_phase(l, q)
                if stop >= 11:
                    diff_phase(l, q)
                if stop >= 12:
                    mlstm_phase(l, q)
            if stop in (10, 11, 12):
                break
            ffn_phase(l, q)
            if stop <= 3:
                break
            ple_phase(l, q)
            if stop <= 4:
                break
            if dbg and q == 0 and l == 0:
                dump_h()
        if stop < 99:
            dump_h()
            break
        store_out(q, NL - 1)
    if record_plan:
        return plan_out
    sch.emit(final_waits=outs)
    return nc


def _t5_bucket(dist):
    max_exact = 16
    d = np.maximum(dist, 1).astype(np.float32)
    large = max_exact + (np.log(d / max_exact) / math.log(128 / max_exact) * (32 - max_exact)).astype(np.int32)
    large = np.minimum(large, 31)
    return np.where(dist < max_exact, dist, large)


def _host_consts():
    r = np.arange(128)
    ident = np.eye(128, dtype=np.float32)
    tri = (r[:, None] <= r[None, :]).astype(np.float32)
    mask = np.where(r[:, None] <= r[None, :], 0.0, NEG).astype(np.float32)
    ones = np.ones((128, 128), np.float32)
    blk = (r[:, None] // 64 == r[None, :] // 64).astype(np.float32)
    sup = (r[:, None] > r[None, :]).astype(np.float32)
    return np.ascontiguousarray(np.concatenate([ident, tri, mask, ones, blk, sup], axis=1))


def _layout_small(inp, NL):
    f = lambda a: np.asarray(a, dtype=np.float32)
    fm = lambda v: f(v).reshape(-1, 128).T
    vecs, rows = [], []
    for l in range(NL):
        cw = f(inp["ssd_conv_w"][l])
        cols = [fm(inp["norm1_w"][l]), fm(inp["norm2_w"][l]), fm(inp["final_norm_w"]), fm(inp["ssd_conv_b"][l]),
                np.concatenate([fm(cw[k]) for k in range(4)], axis=1),
                fm(inp["ssd_norm_w"][l]),
                np.tile(f(inp["diff_norm_w"][l]), 2)[:, None], np.tile(f(inp["diff_norm_w"][l]), 2)[:, None],
                fm(inp["mlstm_norm_w"][l])]
        vecs.append(np.concatenate(cols, axis=1))
        lam = np.concatenate([f(inp["diff_lq1"][l]), f(inp["diff_lq2"][l]), f(inp["diff_lk1"][l]), f(inp["diff_lk2"][l])])
        rw = np.concatenate([f(inp["ssd_dt_bias"][l]), f(inp["ssd_a_log"][l]), f(inp["ssd_d"][l]),
                             f(inp["mlstm_i_bias"][l]), f(inp["mlstm_f_bias"][l]), lam,
                             f(inp["rel_bias"])[31, :]])
        rows.append(np.broadcast_to(rw[None, :], (128, rw.shape[0])))
    return np.ascontiguousarray(np.stack(vecs)), np.ascontiguousarray(np.stack(rows))


def _bias_tiles(rel_bias):
    rb = np.asarray(rel_bias, dtype=np.float32)
    k = np.arange(128)[:, None]
    c = np.arange(256)[None, :]
    dist = c - k
    bucket = _t5_bucket(np.maximum(dist, 0))
    g = rb[bucket]
    g = np.where((dist >= 0)[:, :, None], g, np.float32(NEG))
    return np.ascontiguousarray(np.transpose(g, (0, 2, 1)).reshape(128, 4 * 256))


_CACHE = {}


def _get_nc(S, NSEQ, NL, dbg=False):
    key = (S, NSEQ, NL, dbg)
    if key not in _CACHE:
        plan = build(S, NSEQ, NL, None, dbg)
        _CACHE[key] = build(S, NSEQ, NL, plan, dbg)
    return _CACHE[key]


def run(inp, n_cores, dbg=False):
    x = np.asarray(inp["x"], dtype=np.float32)
    B, S, _ = x.shape
    NL = inp["w_in"].shape[0]
    NSEQ = B // n_cores
    nc = _get_nc(S, NSEQ, NL, dbg)
    vecs, rows = _layout_small(inp, NL)
    consts = _host_consts()
    bt = _bias_tiles(inp["rel_bias"])
    p = np.asarray(inp["p"], dtype=np.float32)
    shared = {"consts": consts, "bt": bt, "vecs": vecs, "rows": rows}
    for k in ("w_in", "w_out", "w_ffn_gate", "w_ffn_up", "w_ffn_down", "ple_gate_w", "ple_proj_w"):
        shared[k] = np.ascontiguousarray(np.asarray(inp[k], dtype=np.float32))
    in_maps = []
    for c in range(n_cores):
        m = dict(shared)
        m["x"] = np.ascontiguousarray(x[c * NSEQ:(c + 1) * NSEQ])
        m["p"] = np.ascontiguousarray(p[:, c * NSEQ:(c + 1) * NSEQ])
        in_maps.append(m)
    res = run_bass_kernel_spmd(nc, in_maps, core_ids=list(range(n_cores)))
    out = np.concatenate([r["out"] for r in res.results], axis=0)
    if dbg:
        return out, [r["dbg"] for r in res.results]
    return out


def kernel(**inputs):
    return run(inputs, 8)
```

```python
import math
import numpy as np
import concourse.bass as bass
import concourse.mybir as mybir
from concourse.bass_utils import run_bass_kernel_spmd
from contextlib import ExitStack

F32 = mybir.dt.float32
BF16 = mybir.dt.bfloat16
AF = mybir.ActivationFunctionType
ALU = mybir.AluOpType

N_DMA_SEMS = 24
NEG = -1.0e30
EPS = 1e-6
IN_COLS = 3344
FFN_H = 2816
O_Z, O_XBC, O_DT, O_DQ, O_DK, O_DV, O_MQ, O_MK, O_MV, O_MO, O_MI = (
    0, 512, 1536, 1544, 1800, 2056, 2312, 2568, 2824, 3080, 3336)


class Buf:
    __slots__ = ("name", "writer", "readers", "excl")

    def __init__(self, name="", excl=False):
        self.name = name
        self.writer = None
        self.readers = []
        self.excl = excl


class Op:
    __slots__ = ("eng", "fn", "deps", "is_dma", "marked", "token", "idx")

    def __init__(self, eng, fn, is_dma):
        self.eng = eng
        self.fn = fn
        self.deps = []
        self.is_dma = is_dma
        self.marked = is_dma
        self.token = None
        self.idx = 0


class Sched:
    ENGS = ("pe", "act", "dve", "pool", "sp")

    def __init__(self, nc):
        self.nc = nc
        self.eng_ops = {e: [] for e in self.ENGS}
        self.dma_ops = []
        self.n_ops = 0

    def op(self, eng, fn, reads=(), writes=(), dma=False):
        o = Op(eng, fn, dma)
        deps = {}
        for b in reads:
            if b.writer is not None:
                deps[id(b.writer)] = b.writer
            if b.excl:
                for r in b.readers:
                    if r.eng != eng:
                        deps[id(r)] = r
        for b in writes:
            if b.writer is not None:
                deps[id(b.writer)] = b.writer
            for r in b.readers:
                deps[id(r)] = r
        if dma:
            if len(self.dma_ops) >= N_DMA_SEMS:
                p = self.dma_ops[len(self.dma_ops) - N_DMA_SEMS]
                deps[id(p)] = p
            o.idx = len(self.dma_ops)
            self.dma_ops.append(o)
        for d in deps.values():
            if d.eng == "pe" and eng == "pe" and not d.is_dma and not dma:
                continue
            d.marked = True
            o.deps.append(d)
        for b in writes:
            b.writer = o
            b.readers = []
        for b in reads:
            if b.writer is not o:
                b.readers.append(o)
        self.eng_ops[eng].append(o)
        self.n_ops += 1
        return o

    def emit(self, final_waits=()):
        nc = self.nc
        with ExitStack() as es:
            esem = {e: es.enter_context(nc.semaphore("s_" + e)) for e in self.ENGS}
            dsem = [es.enter_context(nc.semaphore("s_dma%d" % i)) for i in range(N_DMA_SEMS)]
            cnt = {e: 0 for e in self.ENGS}
            for o in self.dma_ops:
                o.token = (dsem[o.idx % N_DMA_SEMS], 16 * (o.idx // N_DMA_SEMS + 1), 16)
            for e in self.ENGS:
                for o in self.eng_ops[e]:
                    if not o.is_dma and o.marked:
                        cnt[e] += 1
                        o.token = (esem[e], cnt[e], 1)
            block = es.enter_context(nc.Block())

            def run(e, h):
                seen = {}
                for o in self.eng_ops[e]:
                    waits = {}
                    for d in o.deps:
                        s, v, _ = d.token
                        k = id(s)
                        if k not in waits or waits[k][1] < v:
                            waits[k] = (s, v)
                    for k, (s, v) in waits.items():
                        if seen.get(k, 0) >= v:
                            continue
                        h.wait_ge(s, v)
                        seen[k] = v
                    ins = o.fn(h)
                    if o.token is not None:
                        ins.then_inc(o.token[0], o.token[2])
                if e == "sp":
                    waits = {}
                    for d in final_waits:
                        s, v, _ = d.token
                        k = id(s)
                        if k not in waits or waits[k][1] < v:
                            waits[k] = (s, v)
                    for k, (s, v) in waits.items():
                        h.wait_ge(s, v)

            @block.tensor
            def _(h):
                run("pe", h)

            @block.scalar
            def _(h):
                run("act", h)

            @block.vector
            def _(h):
                run("dve", h)

            @block.gpsimd
            def _(h):
                run("pool", h)

            @block.sync
            def _(h):
                run("sp", h)


def build(S, NSEQ, NL, wplan=None, dbg=False):
    NT = S // 128
    NG = S // 512
    nc = bass.Bass("TRN2", target_bir_lowering=False)
    sch = Sched(nc)
    record_plan = wplan is None
    plan_out = []

    def din(name, shape):
        return nc.dram_tensor(name, list(shape), F32, kind="ExternalInput")

    x_d = din("x", [NSEQ, S, 1024])
    p_d = din("p", [NL, NSEQ, S, 256])
    win_d = din("w_in", [NL, 1024, IN_COLS])
    wout_d = din("w_out", [NL, 1024, 1024])
    wg_d = din("w_ffn_gate", [NL, 1024, FFN_H])
    wu_d = din("w_ffn_up", [NL, 1024, FFN_H])
    wd_d = din("w_ffn_down", [NL, FFN_H, 1024])
    wpg_d = din("ple_gate_w", [NL, 1024, 1024])
    wpp_d = din("ple_proj_w", [NL, 256, 1024])
    cst_d = din("consts", [128, 6 * 128])
    bt_d = din("bt", [128, 4 * 256])
    NV = 8 + 8 + 8 + 8 + 32 + 4 + 2 + 2
    NR = 8 + 8 + 8 + 4 + 4 + 128 + 4
    vec_d = din("vecs", [NL, 128, NV])
    row_d = din("rows", [NL, 128, NR])
    out_d = nc.dram_tensor("out", [NSEQ, S, 1024], F32, kind="ExternalOutput")
    dbg_d = nc.dram_tensor("dbg", [128, 8, S], F32, kind="ExternalOutput") if dbg else None

    hT = nc.alloc_sbuf_tensor("hT", [128, 8, S], F32)
    hB = [[Buf() for _ in range(NG)] for _ in range(8)]
    uT = nc.alloc_sbuf_tensor("uT", [128, 8, S], BF16)
    uB = [[Buf() for _ in range(NG)] for _ in range(8)]
    NU = 61
    NF = 16
    arena = nc.alloc_sbuf_tensor("arena", [128, NU, 512], BF16)
    aB = [Buf("a%d" % i) for i in range(NU)]
    farena = nc.alloc_sbuf_tensor("farena", [128, NF, 256], F32)
    fB = [Buf("f%d" % i) for i in range(NF)]
    NW = 2
    wts = [nc.alloc_sbuf_tensor("wt%d" % i, [128, 8, 512], BF16) for i in range(NW)]
    wB = [Buf() for _ in range(NW)]
    wstg = [nc.alloc_sbuf_tensor("wstg%d" % i, [128, 1, 512], F32) for i in range(2)]
    wsB = [Buf() for _ in range(2)]
    cst = nc.alloc_sbuf_tensor("cst", [128, 6 * 128], F32)
    cB = Buf()
    cbf = nc.alloc_sbuf_tensor("cbf", [128, 3 * 128 + 260], BF16)
    cbB = Buf()
    vec = nc.alloc_sbuf_tensor("vec", [128, NL, NV], F32)
    row = nc.alloc_sbuf_tensor("row", [128, NL, NR], F32)
    vrB = Buf()
    small = nc.alloc_sbuf_tensor("small", [128, 64], F32)
    smB = Buf()
    smP = [[Buf() for _ in range(3)] for _ in range(2)]
    gates = nc.alloc_sbuf_tensor("gates", [128, 3, NT, 8], F32)
    gB = [Buf() for _ in range(4)]
    ssdS = nc.alloc_sbuf_tensor("ssdS", [128, 8, 64], F32)
    ssdSb = nc.alloc_sbuf_tensor("ssdSb", [128, 8, 64], BF16)
    mlS = nc.alloc_sbuf_tensor("mlS", [128, 2, 2, 64], F32)
    mlSb = nc.alloc_sbuf_tensor("mlSb", [128, 2, 2, 64], BF16)
    stB = [Buf() for _ in range(4)]
    banks = [nc.alloc_psum_tensor("bank%d" % i, [128, 512], F32) for i in range(8)]
    bkB = [Buf(excl=True) for _ in range(8)]
    st = {"ps": 0, "w": 0, "wq": 0, "ws": 0}

    ident_f = cst[:, 0:128]
    tri_f = cst[:, 128:256]
    mask_f = cst[:, 256:384]
    ones_f = cst[:, 384:512]
    sup_f = cst[:, 640:768]
    ident_b = cbf[:, 0:128]
    ones_b = cbf[:, 128:256]
    blk_b = cbf[:, 256:384]
    zero_b = cbf[:, 384:512]

    def ps():
        i = st["ps"] % 6
        st["ps"] += 1
        return banks[i], bkB[i]

    def ps_held(j):
        return banks[6 + j], bkB[6 + j]

    def abf(u, n=1):
        return arena[:, u:u + n, :].rearrange("p a b -> p (a b)")

    def ff(u, n):
        return farena[:, u:u + n, :].rearrange("p a b -> p (a b)")

    def fb(u, n=1):
        return fB[u:u + n]

    F_RT, F_TMP, F_X = 0, 4, 12

    def ab(u, n=1):
        return aB[u:u + n]

    def ACT(out, in_, func, reads, writes, bias=None, scale=None):
        kw = {}
        if bias is not None:
            kw["bias"] = bias
        if scale is not None:
            kw["scale"] = scale
        sch.op("act", lambda h: h.activation(out=out, in_=in_, func=func, **kw), reads, writes)

    def TT(eng, out, in0, in1, op, reads, writes):
        sch.op(eng, lambda h: h.tensor_tensor(out=out, in0=in0, in1=in1, op=op), reads, writes)

    def TS(eng, out, in0, s1, op0, reads, writes, s2=None, op1=None):
        if op1 is None:
            sch.op(eng, lambda h: h.tensor_scalar(out=out, in0=in0, scalar1=s1, scalar2=None, op0=op0), reads, writes)
        else:
            sch.op(eng, lambda h: h.tensor_scalar(out=out, in0=in0, scalar1=s1, scalar2=s2, op0=op0, op1=op1), reads, writes)

    def STT(out, in0, scalar, in1, op0, op1, reads, writes):
        sch.op("dve", lambda h: h.scalar_tensor_tensor(out=out, in0=in0, scalar=scalar, in1=in1, op0=op0, op1=op1), reads, writes)

    def CP(eng, out, in_, reads, writes):
        if eng == "act":
            sch.op("act", lambda h: h.copy(out=out, in_=in_), reads, writes)
        else:
            sch.op(eng, lambda h: h.tensor_copy(out=out, in_=in_), reads, writes)

    def MSET(eng, ap, val, writes):
        sch.op(eng, lambda h: h.memset(ap, val), (), writes)

    def RECIP(out, in_, reads, writes):
        sch.op("dve", lambda h: h.reciprocal(out=out, in_=in_), reads, writes)

    def MMG(mms, reads, writes):
        def fn(h):
            ins = None
            for m in mms:
                kw = {}
                if m.get("tp") is not None:
                    kw["tile_position"] = m["tp"]
                if m.get("sgc"):
                    kw["skip_group_check"] = True
                ins = h.matmul(m["out"], lhsT=m["lhsT"], rhs=m["rhs"], start=m["start"], stop=m["stop"], **kw)
            return ins
        sch.op("pe", fn, reads, writes)

    def TRG(trs, reads, writes):
        def fn(h):
            ins = None
            for (o, i, idn) in trs:
                ins = h.transpose(out=o, in_=i, identity=idn, tile_position=(0, 0))
            return ins
        sch.op("pe", fn, reads, writes)

    def DMA(eng, out, in_, reads, writes):
        return sch.op(eng, lambda h: h.dma_start(out=out, in_=in_), reads, writes, dma=True)

    wspecs = wplan if wplan is not None else []

    def w_issue(k):
        (name, l, r0, nr, c0, ncw) = wspecs[k]
        src_t = {"w_in": win_d, "w_out": wout_d, "wg": wg_d, "wu": wu_d, "wd": wd_d, "wpg": wpg_d, "wpp": wpp_d}[name]
        kch = nr // 128
        src = src_t[l, r0:r0 + nr, c0:c0 + ncw].rearrange("(k p) n -> p k n", p=128)
        i = k % NW
        for k0 in range(0, kch, 1):
            nk = 1
            j = st["ws"] % 2
            st["ws"] += 1
            DMA("sp", wstg[j][:, 0:nk, 0:ncw], src[:, k0:k0 + nk, :], (), [wsB[j]])
            CP("pool", wts[i][:, k0:k0 + nk, 0:ncw], wstg[j][:, 0:nk, 0:ncw], [wsB[j]], [wB[i]])

    def W(name, l, r0, nr, c0, ncw, hold=0):
        spec = (name, l, r0, nr, c0, ncw)
        k = st["w"]
        st["w"] += 1
        if record_plan:
            plan_out.append(spec)
            return wts[0], wB[0]
        assert wspecs[k] == spec, (k, wspecs[k], spec)
        while st["wq"] < min(len(wspecs), k + NW - hold):
            w_issue(st["wq"])
            st["wq"] += 1
        return wts[k % NW], wB[k % NW]

    def prologue():
        DMA("sp", cst[:], cst_d[:], (), [cB])
        DMA("sp", vec[:], vec_d.ap().rearrange("l p n -> p l n"), (), [vrB])
        DMA("sp", row[:], row_d.ap().rearrange("l p n -> p l n"), (), [vrB])
        CP("dve", cbf[:, 0:128], cst[:, 0:128], [cB], [cbB])
        CP("dve", cbf[:, 128:256], cst[:, 384:512], [cB], [cbB])
        CP("dve", cbf[:, 256:384], cst[:, 512:640], [cB], [cbB])
        MSET("dve", cbf[:, 384:644], 0.0, [cbB])
        MSET("dve", small[:, 0:1], EPS, [smB])
        MSET("dve", small[:, 1:2], 1.0, [smB])

    def vcol(l, off, n=1):
        return vec[:, l, off:off + n]

    V_N1, V_N2, V_NF, V_CB, V_CW, V_SN, V_DN, V_MN = 0, 8, 16, 24, 32, 64, 68, 70
    R_DTB, R_ALOG, R_DSK, R_IB, R_FB, R_LAM, R_FAR, R_DSKX = 0, 8, 16, 24, 28, 32, 160, 164

    SCR = 48

    def rstd_from_ss(ssb, ssB, rt, rtB, n, divisor):
        ACT(rt, ssb, AF.Sqrt, [ssB, smB], rtB, bias=small[:, 0:1], scale=1.0 / divisor)
        RECIP(rt, rt, rtB, rtB)

    def rmsnorm_h(wc, dst, dstB, dst_f32=False):
        for g in range(NG):
            sl = slice(g * 512, (g + 1) * 512)
            ssb, ssB = ps()
            for c in range(8):
                u = SCR + (c % 2)
                ACT(abf(u), hT[:, c, sl], AF.Square, [hB[c][g]], ab(u))
                MMG([dict(out=ssb[:], lhsT=ones_b, rhs=abf(u), start=(c == 0), stop=(c == 7))], ab(u) + [cbB], [ssB])
            ru = F_RT + 2 * (g % 2)
            rt = ff(ru, 2)
            rstd_from_ss(ssb[:], ssB, rt, fb(ru, 2), 512, 1024.0)
            for c in range(8):
                STT(dst(c, g), hT[:, c, sl], wc[:, c:c + 1], rt, ALU.mult, ALU.mult,
                    [hB[c][g], vrB] + fb(ru, 2), dstB(c, g))

    def u_ap(c, g):
        return uT[:, c, g * 512:(g + 1) * 512]

    def u_b(c, g):
        return [uB[c][g]]

    def dense_fm(wt, wbuf, kch, col0, rhs_ap, rhs_bufs, g, ncol=128):
        pb, pbB = ps()
        mms = [dict(out=pb[0:ncol, :], lhsT=wt[:, k, col0:col0 + ncol], rhs=rhs_ap(k, g),
                    start=(k == 0), stop=(k == kch - 1)) for k in range(kch)]
        rb = [wbuf]
        for k in range(kch):
            rb += rhs_bufs(k, g)
        MMG(mms, rb, [pbB])
        return pb, pbB

    def add_to_h(c, g, pb, pbB):
        sl = slice(g * 512, (g + 1) * 512)
        TT("dve", hT[:, c, sl], hT[:, c, sl], pb[:], ALU.add, [hB[c][g], pbB], [hB[c][g]])

    def wout_partial(l, k0, nk, src_ap, src_bufs):
        for half in range(2):
            wt, wbuf = W("w_out", l, k0 * 128, nk * 128, half * 512, 512)
            for cc in range(4):
                c = half * 4 + cc
                for g in range(NG):
                    pb, pbB = dense_fm(wt, wbuf, nk, cc * 128, src_ap, src_bufs, g)
                    add_to_h(c, g, pb, pbB)

    def post_norm(u0, nch, mode, wcol, gate=None):
        for g in range(NG):
            for c in range(nch):
                u = u0 + c * NG + g
                if mode == "all" and c > 0:
                    pass
                else:
                    ssb, ssB = ps()
                if mode == "all":
                    squ = SCR + (c % 2)
                    ACT(abf(squ), abf(u), AF.Square, ab(u), ab(squ))
                    MMG([dict(out=ssb[:], lhsT=ones_b, rhs=abf(squ), start=(c == 0), stop=(c == nch - 1))],
                        ab(squ) + [cbB], [ssB])
                    if c < nch - 1:
                        continue
                    ru = F_RT + 2 * (g % 2)
                    rstd_from_ss(ssb[:], ssB, ff(ru, 2), fb(ru, 2), 512, nch * 128.0)
                    for c2 in range(nch):
                        u2 = u0 + c2 * NG + g
                        STT(abf(u2), abf(u2), wcol[:, c2:c2 + 1], ff(ru, 2), ALU.mult, ALU.mult,
                            ab(u2) + fb(ru, 2) + [vrB, smB], ab(u2))
                else:
                    squ = SCR + (c % 2)
                    ACT(abf(squ), abf(u), AF.Square, ab(u), ab(squ))
                    MMG([dict(out=ssb[:], lhsT=blk_b, rhs=abf(squ), start=True, stop=True)], ab(squ) + [cbB], [ssB])
                    ru = F_RT + 2 * (c % 2)
                    rstd_from_ss(ssb[:], ssB, ff(ru, 2), fb(ru, 2), 512, 64.0)
                    STT(abf(u), abf(u), wcol[:, c:c + 1], ff(ru, 2), ALU.mult, ALU.mult,
                        ab(u) + fb(ru, 2) + [vrB, smB], ab(u))
                    if gate is not None:
                        gate(c, g, u)

    U_D, U_E, U_MT, U_CP, U_XB, U_XP, U_XD, U_XPP = (
        SCR + 2, SCR + 3, SCR + 4, SCR + 5, SCR + 6, SCR + 8, SCR + 9, SCR + 10)

    def decay_unit(par, adt_ap, adtB, nh_tot, hsel):
        h0 = hsel
        ru = (F_X, F_TMP)[par]
        rhs = ff(ru, 2).rearrange("p (h l) -> p h l", h=4)
        TT("pool", rhs, tri_f.unsqueeze(1).to_broadcast([128, 4, 128]),
           adt_ap[:, h0:h0 + 4].unsqueeze(2).to_broadcast([128, 4, 128]), ALU.mult, [cB] + adtB, fb(ru, 2))
        Rb, RB = ps()
        MMG([dict(out=Rb[:], lhsT=ones_f, rhs=ff(ru, 2), start=True, stop=True)], fb(ru, 2) + [cB], [RB])
        Tb, TB = ps()
        MMG([dict(out=Tb[:], lhsT=sup_f, rhs=ff(ru, 2), start=True, stop=True)], fb(ru, 2) + [cB], [TB])
        so = 8 + 16 * par
        tu = (F_X + 2, F_TMP + 2)[par]
        T1 = ff(tu, 2).rearrange("p (h l) -> p h l", h=4)
        R3 = Rb[:].rearrange("p (h l) -> p h l", h=4)
        TT("dve", T1, Tb[:].rearrange("p (h l) -> p h l", h=4), mask_f.unsqueeze(1).to_broadcast([128, 4, 128]),
           ALU.add, [TB, cB], fb(tu, 2))
        du = (U_D, SCR + 0)[par]
        D = abf(du).rearrange("p (h l) -> p h l", h=4)
        ACT(D, T1, AF.Exp, fb(tu, 2), ab(du))
        dlast = small[:, so + 4:so + 8]
        ACT(dlast, T1[:, :, 127], AF.Exp, fb(tu, 2), [smP[par][1]])
        eu = (U_E, SCR + 1)[par]
        E = abf(eu).rearrange("p (h l) -> p h l", h=4)
        ACT(E, R3, AF.Exp, [RB], ab(eu))
        elast = small[:, so + 8:so + 12]
        ACT(elast, R3[:, :, 127], AF.Exp, [RB], [smP[par][2]])
        return D, ab(du), E, ab(eu), dlast, elast

    def ssd_phase(l, q):
        YG, XBC = 0, 16
        ST = SCR
        for half in range(2):
            wt, wbuf = W("w_in", l, 0, 1024, O_XBC + half * 512, 512)
            for cc in range(4):
                j = half * 4 + cc
                sg = ST + 5 * (j % 2)
                stage = arena[:, sg:sg + 5, :].rearrange("p a b -> p (a b)")
                du = SCR + 10 + (j % 2)
                dg = abf(du).rearrange("p (k c) -> p k c", k=4)
                for k in range(4):
                    TS("dve", dg[:, k, :], ident_f, vcol(l, V_CW + k * 8 + j), ALU.mult, [cB, vrB], ab(du))
                MSET("dve", stage[:, 0:3], 0.0, ab(sg))
                for g in range(NG):
                    pb, pbB = dense_fm(wt, wbuf, 8, cc * 128, lambda k, g_: u_ap(k, g_), lambda k, g_: u_b(k, g_), g)
                    CP("act", stage[:, 3 + g * 512:3 + (g + 1) * 512], pb[:], [pbB], ab(sg + g) + ab(sg + g + 1))
                for g in range(NG):
                    cb_, cbB_ = ps()
                    mms = [dict(out=cb_[:], lhsT=dg[:, k, :], rhs=stage[:, g * 512 + k:g * 512 + k + 512],
                                start=(k == 0), stop=(k == 3)) for k in range(4)]
                    MMG(mms, ab(du) + ab(sg + g) + ab(sg + g + 1) + (ab(sg + g - 1) if g > 0 else []), [cbB_])
                    u = XBC + j * NG + g
                    ACT(abf(u), cb_[:], AF.Silu, [cbB_, vrB], ab(u), bias=vcol(l, V_CB + j))
        wt, wbuf = W("w_in", l, 0, 1024, O_DT, 8)
        pb, pbB = ps()
        for t in range(NT):
            mms = [dict(out=pb[:, t * 8:(t + 1) * 8], lhsT=uT[:, k, t * 128:(t + 1) * 128], rhs=wt[:, k, 0:8],
                        start=(k == 0), stop=(k == 7)) for k in range(8)]
            MMG(mms, [wbuf] + [uB[k][t // 4] for k in range(8)], [pbB])
        dt3 = gates[:, 0, :, :]
        adt3 = gates[:, 1, :, :]
        TT("dve", dt3, pb[:, 0:NT * 8].rearrange("p (t h) -> p t h", h=8),
           row[:, l, R_DTB:R_DTB + 8].unsqueeze(1).to_broadcast([128, NT, 8]), ALU.add, [pbB, vrB], [gB[0]])
        ACT(dt3, dt3, AF.Exp, [gB[0]], [gB[0]])
        ACT(dt3, dt3, AF.Ln, [gB[0], smB], [gB[0]], bias=small[:, 1:2])
        ACT(small[:, 40:48], row[:, l, R_ALOG:R_ALOG + 8], AF.Exp, [vrB], [smB])
        STT(adt3, dt3, -1.0, small[:, 40:48].unsqueeze(1).to_broadcast([128, NT, 8]), ALU.mult, ALU.mult,
            [gB[0], smB], [gB[1]])
        MSET("dve", ssdS[:], 0.0, [stB[0]])
        MSET("dve", ssdSb[:], 0.0, [stB[1]])
        dskx = row[:, l, R_DSK:R_DSK + 8].unsqueeze(2).to_broadcast([128, 8, 64])
        for c in range(NT):
            g = c // 4
            off = (c % 4) * 128
            par = c % 2

            def xb_ap(j):
                return abf(XBC + j * NG + g)[:, off:off + 128]

            tb, tbB = ps()
            tbv = tb[:].bitcast(BF16)
            TRG([(tbv[:, j * 128:(j + 1) * 128], xb_ap(j), ident_b) for j in range(6)],
                [aB[XBC + j * NG + g] for j in range(6)] + [cbB], [tbB])
            xu = U_XB
            xB_ = abf(xu, 2)[:, 0:768]
            CP("act", xB_, tbv[:, 0:768], [tbB], ab(xu, 2))
            xs3 = xB_[:, 0:512].rearrange("p (h e) -> p h e", h=8)
            xpu, xdu, xppu = U_XP, U_XD, U_XPP
            Xp = abf(xpu).rearrange("p (h e) -> p h e", h=8)
            Xd = abf(xdu).rearrange("p (h e) -> p h e", h=8)
            Xpp = abf(xppu).rearrange("p (h e) -> p h e", h=8)
            TT("dve", Xp, xs3, gates[:, 0, c, :].unsqueeze(2).to_broadcast([128, 8, 64]), ALU.mult,
               ab(xu, 2) + [gB[0]], ab(xpu))
            TT("pool", Xd, xs3, dskx, ALU.mult, ab(xu, 2) + [vrB], ab(xdu))
            gr = []
            for grp in range(2):
                D, DB, E, EB, dlast, elast = decay_unit(grp, gates[:, 1, c, :], [gB[1]], 8, grp * 4)
                Gb, GB = ps()
                MMG([dict(out=Gb[:, 0:128], lhsT=xb_ap(4 + grp), rhs=xb_ap(6 + grp), start=True, stop=True)],
                    [aB[XBC + (4 + grp) * NG + g], aB[XBC + (6 + grp) * NG + g]], [GB])
                mu = (U_MT, SCR + 11)[grp]
                MT = abf(mu).rearrange("p (h l) -> p h l", h=4)
                TT("dve", MT, D, Gb[:, 0:128].unsqueeze(1).to_broadcast([128, 4, 128]), ALU.mult, DB + [GB], ab(mu))
                cu = (U_CP, SCR + 12)[grp]
                CpT = abf(cu).rearrange("p (h l) -> p h l", h=4)
                TT("pool", CpT, E, xb_ap(6 + grp).unsqueeze(1).to_broadcast([128, 4, 128]), ALU.mult,
                   EB + [aB[XBC + (6 + grp) * NG + g]], ab(cu))
                gr.append((MT, mu, CpT, cu, dlast, elast))
            for grp in range(2):
                MT, mu, CpT, cu, dlast, elast = gr[grp]
                Yb, YB = ps()
                Y4 = Yb[:].rearrange("p (t l) -> p t l", t=4)
                mms = []
                for hh in range(4):
                    hd = grp * 4 + hh
                    o = Y4[(hd % 2) * 64:(hd % 2) * 64 + 64, hh // 2, :]
                    mms.append(dict(out=o, lhsT=Xd[:, hd, :], rhs=ident_b, start=True, stop=False))
                    mms.append(dict(out=o, lhsT=Xp[:, hd, :], rhs=MT[:, hh, :], start=False, stop=False))
                    mms.append(dict(out=o, lhsT=ssdSb[:, hd, :], rhs=CpT[:, hh, :], start=False, stop=True))
                MMG(mms, ab(xdu) + ab(xpu) + ab(mu) + ab(cu) + [stB[1], cbB], [YB])
                for jj in range(2):
                    u = YG + (grp * 2 + jj) * NG + g
                    CP("act" if jj else "dve", abf(u)[:, off:off + 128], Y4[:, jj, :], [YB], ab(u))
                TT("dve", Xpp[:, grp * 4:grp * 4 + 4, :], Xp[:, grp * 4:grp * 4 + 4, :],
                   dlast.unsqueeze(2).to_broadcast([128, 4, 64]), ALU.mult, ab(xpu) + [smP[grp][1]], ab(xppu))
                Sb_, SB_ = ps()
                S3 = Sb_[:, 0:256].rearrange("p (h e) -> p h e", h=4)
                mms = [dict(out=S3[:, hh, :], lhsT=xB_[:, 512 + grp * 128:512 + (grp + 1) * 128],
                            rhs=Xpp[:, grp * 4 + hh, :], start=True, stop=True) for hh in range(4)]
                MMG(mms, ab(xu, 2) + ab(xppu), [SB_])
                Sg = ssdS[:, grp * 4:grp * 4 + 4, :]
                TT("pool", Sg, Sg, elast.unsqueeze(2).to_broadcast([128, 4, 64]), ALU.mult, [stB[0], smP[grp][2]], [stB[0]])
                TT("dve", Sg, Sg, S3, ALU.add, [stB[0], SB_], [stB[0]])
                CP("act", ssdSb[:, grp * 4:grp * 4 + 4, :], Sg, [stB[0]], [stB[1]])
        import os
        if os.environ.get("SSD_STEP") == "1":
            return
        wt, wbuf = W("w_in", l, 0, 1024, O_Z, 512)
        for j in range(4):
            for g in range(NG):
                pb, pbB = dense_fm(wt, wbuf, 8, j * 128, lambda k, g_: u_ap(k, g_), lambda k, g_: u_b(k, g_), g)
                su = F_TMP + 2 * ((j * NG + g) % 2)
                ACT(ff(su, 2), pb[:], AF.Silu, [pbB], fb(su, 2))
                u = YG + j * NG + g
                TT("dve", abf(u), abf(u), ff(su, 2), ALU.mult, ab(u) + fb(su, 2), ab(u))
        post_norm(YG, 4, "all", vcol(l, V_SN, 4))
        wout_partial(l, 0, 4, lambda k, g_: abf(YG + k * NG + g_), lambda k, g_: ab(YG + k * NG + g_))

    def diff_phase(l, q):
        OD, QT, KT, VP, BT = 0, 16, 24, 32, 42
        SCALE = 32 ** -0.5
        btile = ff(F_X, 4).rearrange("p (h c) -> p h c", h=4)
        DMA("sp", ff(F_X, 4), bt_d[:], (), fb(F_X, 4))
        lam_init = 0.8 - 0.6 * math.exp(-0.3 * l)
        lr = row[:, l, R_LAM:R_LAM + 128]
        pr = ff(F_RT, 1)
        TT("dve", pr[:, 0:64], lr[:, 0:64], lr[:, 64:128], ALU.mult, [vrB], fb(F_RT))
        sch.op("dve", lambda h: h.tensor_reduce(out=small[:, 48:50], in_=pr[:, 0:64].rearrange("p (a b) -> p a b", a=2),
                                                axis=mybir.AxisListType.X, op=ALU.add), fb(F_RT), [smB])
        ACT(small[:, 48:50], small[:, 48:50], AF.Exp, [smB], [smB])
        TT("dve", small[:, 50:51], small[:, 48:49], small[:, 49:50], ALU.subtract, [smB], [smB])
        TS("dve", small[:, 51:52], small[:, 50:51], -1.0, ALU.mult, [smB], [smB], s2=-lam_init, op1=ALU.add)
        TS("dve", small[:, 52:53], vcol(l, V_DN), 1.0 - lam_init, ALU.mult, [vrB], [smB])
        wt, wbuf = W("w_in", l, 0, 1024, O_DQ, 512)
        for j in range(4):
            for g in range(NG):
                pb, pbB = dense_fm(wt, wbuf, 8, j * 128, lambda k, g_: u_ap(k, g_), lambda k, g_: u_b(k, g_), g)
                u = (QT if j < 2 else KT) + (j % 2) * NG + g
                CP("act" if (j + g) % 2 else "dve", abf(u), pb[:], [pbB], ab(u))
        wt, wbuf = W("w_in", l, 0, 1024, O_DV, 256)
        VPn = (NT * 260 + 511) // 512
        vp = abf(VP, VPn)[:, 0:NT * 260].rearrange("p (t h e) -> p t h e", t=NT, h=4)
        MSET("pool", vp[:, :, :, 64:65], 1.0, ab(VP, VPn))
        for t in range(NT):
            pb, pbB = ps()
            mms = [dict(out=pb[:, 0:256], lhsT=uT[:, k, t * 128:(t + 1) * 128], rhs=wt[:, k, 0:256],
                        start=(k == 0), stop=(k == 7)) for k in range(8)]
            MMG(mms, [wbuf] + [uB[k][t // 4] for k in range(8)], [pbB])
            CP("act" if t % 2 else "dve", vp[:, t, :, 0:64], pb[:, 0:256].rearrange("p (h e) -> p h e", h=4),
               [pbB], ab(VP, VPn))
        PT0 = SCR + 2
        TM0 = F_TMP
        OT = F_TMP + 4
        ring = {"p": 0, "t": 0}
        for G in range(NG):
            nq = 4
            for hd in range(4):
                accs = []
                for m in range(2):
                    ac, acB = ps_held(m)
                    MMG([dict(out=ac[:, 0:260], lhsT=zero_b, rhs=cbf[:, 384:644], start=True, stop=False, sgc=True)],
                        [cbB], [acB])
                    accs.append((ac, acB))
                ch = hd // 2
                steps = [(i, m) for i in range(4 * G + 4) for m in range(2)]
                pend = {}

                def issue_S(k):
                    i, m = steps[k]
                    pbase = ((hd % 2) * 2 + m) * 32
                    kt_ap = abf(KT + ch * NG + i // 4)[pbase:pbase + 32, (i % 4) * 128:(i % 4) * 128 + 128]
                    qt_ap = abf(QT + ch * NG + G)[pbase:pbase + 32, :]
                    sb, sB = ps()
                    MMG([dict(out=sb[:], lhsT=kt_ap, rhs=qt_ap, start=True, stop=True, tp=(pbase, 0))],
                        [aB[KT + ch * NG + i // 4], aB[QT + ch * NG + G]], [sB])
                    pend[k] = (sb, sB)

                issue_S(0)
                issue_S(1)
                for k in range(len(steps)):
                    i, m = steps[k]
                    sb, sB = pend.pop(k)
                    pu = PT0 + ring["p"] % 4
                    ring["p"] += 1
                    PT = abf(pu)
                    jl0 = max(0, i - 4 * G)
                    far0 = max(jl0, i + 2 - 4 * G)
                    for jl in range(jl0, min(far0, nq)):
                        dlt = 4 * G + jl - i
                        tu = TM0 + (ring["t"] % 4)
                        ring["t"] += 1
                        tmp = ff(tu, 1)[:, 0:128]
                        STT(tmp, sb[:, jl * 128:(jl + 1) * 128], SCALE, btile[:, hd, dlt * 128:(dlt + 1) * 128],
                            ALU.mult, ALU.add, [sB] + fb(F_X, 4), fb(tu))
                        ACT(PT[:, jl * 128:(jl + 1) * 128], tmp, AF.Exp, fb(tu), ab(pu))
                    if far0 < nq:
                        ACT(PT[:, far0 * 128:nq * 128], sb[:, far0 * 128:nq * 128], AF.Exp, [sB, vrB], ab(pu),
                            bias=row[:, l, R_FAR + hd:R_FAR + hd + 1], scale=SCALE)
                    if k + 2 < len(steps):
                        issue_S(k + 2)
                    ac, acB = accs[m]
                    mms = [dict(out=ac[:, jl * 65:(jl + 1) * 65], lhsT=PT[:, jl * 128:(jl + 1) * 128],
                                rhs=vp[:, i, hd, :], start=False, stop=False, sgc=True) for jl in range(jl0, nq)]
                    MMG(mms, ab(pu) + ab(VP, VPn), [acB])
                a0, a0B = accs[0]
                a1, a1B = accs[1]
                A0 = a0[:, 0:260].rearrange("p (j e) -> p j e", j=4)
                A1 = a1[:, 0:260].rearrange("p (j e) -> p j e", j=4)
                so = 24
                RECIP(small[:, so:so + 4], A0[:, :, 64], [a0B], [smB])
                RECIP(small[:, so + 4:so + 8], A1[:, :, 64], [a1B], [smB])
                TS("dve", small[:, so + 4:so + 8], small[:, so + 4:so + 8], small[:, 51:52], ALU.mult, [smB], [smB])
                for jl in range(4):
                    t = 4 * G + jl
                    ou = OT + jl
                    o3 = ff(ou, 1).rearrange("p (h e) -> p h e", h=4)
                    if hd == 0:
                        pass
                    TS("dve", o3[:, hd, :], A0[:, jl, 0:64], small[:, so + jl:so + jl + 1], ALU.mult, [a0B, smB], fb(ou))
                    STT(o3[:, hd, :], A1[:, jl, 0:64], small[:, so + 4 + jl:so + 5 + jl], o3[:, hd, :], ALU.mult, ALU.add,
                        [a1B, smB] + fb(ou), fb(ou))
                    if hd == 3:
                        tb, tbB = ps()
                        TRG([(tb[:, j2 * 128:(j2 + 1) * 128], ff(ou, 1)[:, j2 * 128:(j2 + 1) * 128], ident_f)
                             for j2 in range(2)], fb(ou) + [cB], [tbB])
                        for j2 in range(2):
                            u = OD + j2 * NG + G
                            CP("act", abf(u)[:, jl * 128:(jl + 1) * 128], tb[:, j2 * 128:(j2 + 1) * 128], [tbB], ab(u))
        post_norm(OD, 2, "blk", small[:, 52:53].to_broadcast([128, 2]))
        wout_partial(l, 4, 2, lambda k, g_: abf(OD + k * NG + g_), lambda k, g_: ab(OD + k * NG + g_))

    def mlstm_phase(l, q):
        HM, VT, QT, KT, KTK = 0, 8, 16, 32, 40
        wt, wbuf = W("w_in", l, 0, 1024, O_MQ, 512)
        for j in range(4):
            for g in range(NG):
                pb, pbB = dense_fm(wt, wbuf, 8, j * 128, lambda k, g_: u_ap(k, g_), lambda k, g_: u_b(k, g_), g)
                if j < 2:
                    for hf in range(2):
                        u = QT + (2 * j + hf) * NG + g
                        r0, z0 = hf * 64, (1 - hf) * 64
                        CP("act", abf(u)[r0:r0 + 64, :], pb[r0:r0 + 64, :], [pbB], ab(u))
                        MSET("pool", abf(u)[z0:z0 + 64, :], 0.0, ab(u))
                else:
                    u = KT + (j % 2) * NG + g
                    TS("dve", abf(u), pb[:], 0.125, ALU.mult, [pbB], ab(u))
        wt, wbuf = W("w_in", l, 0, 1024, O_MV, 256)
        vt = abf(VT, NT // 2 if NT >= 2 else 1)[:, 0:NT * 256].rearrange("p (t h e) -> p t h e", t=NT, h=4)
        VTn = max(1, NT // 2)
        for t in range(NT):
            pb, pbB = ps()
            mms = [dict(out=pb[:, 0:256], lhsT=uT[:, k, t * 128:(t + 1) * 128], rhs=wt[:, k, 0:256],
                        start=(k == 0), stop=(k == 7)) for k in range(8)]
            MMG(mms, [wbuf] + [uB[k][t // 4] for k in range(8)], [pbB])
            CP("act" if t % 2 else "dve", vt[:, t, :, :], pb[:, 0:256].rearrange("p (h e) -> p h e", h=4),
               [pbB], ab(VT, VTn))
        wt, wbuf = W("w_in", l, 0, 1024, O_MK, 256)
        ktk = abf(KTK, VTn)[:, 0:NT * 256].rearrange("p (t c) -> p t c", t=NT)
        for t in range(NT):
            pb, pbB = ps()
            mms = [dict(out=pb[:, 0:256], lhsT=uT[:, k, t * 128:(t + 1) * 128], rhs=wt[:, k, 0:256],
                        start=(k == 0), stop=(k == 7)) for k in range(8)]
            MMG(mms, [wbuf] + [uB[k][t // 4] for k in range(8)], [pbB])
            TS("dve", ktk[:, t, :], pb[:, 0:256], 0.125, ALU.mult, [pbB], ab(KTK, VTn))
        wt, wbuf = W("w_in", l, 0, 1024, O_MI, 8)
        pb, pbB = ps()
        for t in range(NT):
            mms = [dict(out=pb[:, t * 8:(t + 1) * 8], lhsT=uT[:, k, t * 128:(t + 1) * 128], rhs=wt[:, k, 0:8],
                        start=(k == 0), stop=(k == 7)) for k in range(8)]
            MMG(mms, [wbuf] + [uB[k][t // 4] for k in range(8)], [pbB])
        g3 = gates[:, 2, :, :]
        TT("dve", g3, pb[:, 0:NT * 8].rearrange("p (t h) -> p t h", h=8),
           row[:, l, R_IB:R_IB + 8].unsqueeze(1).to_broadcast([128, NT, 8]), ALU.add, [pbB, vrB], [gB[2]])
        ACT(g3[:, :, 0:4], g3[:, :, 0:4], AF.Exp, [gB[2]], [gB[2]])
        ACT(g3[:, :, 4:8], g3[:, :, 4:8], AF.Exp, [gB[2]], [gB[2]], scale=-1.0)
        ACT(g3[:, :, 4:8], g3[:, :, 4:8], AF.Ln, [gB[2], smB], [gB[2]], bias=small[:, 1:2])
        TS("dve", g3[:, :, 4:8], g3[:, :, 4:8], -1.0, ALU.mult, [gB[2]], [gB[2]])
        MSET("dve", mlS[:], 0.0, [stB[2]])
        MSET("dve", mlSb[:], 0.0, [stB[3]])
        import os
        for c in range(int(os.environ.get("ML_NCH", NT))):
            g = c // 4
            off = (c % 4) * 128
            par = c % 2

            def q_ap(hd):
                return abf(QT + hd * NG + g)[:, off:off + 128]

            def k_ap(j):
                return abf(KT + j * NG + g)[:, off:off + 128]

            ktok = ktk[:, c, :]
            xpu, xppu = U_XP, U_XPP
            Xp = abf(xpu).rearrange("p (h s e) -> p h s e", h=4, s=2)
            Xpp = abf(xppu).rearrange("p (h s e) -> p h s e", h=4, s=2)
            ei = gates[:, 2, c, 0:4]
            TT("dve", Xp[:, :, 0, :], vt[:, c, :, :], ei.unsqueeze(2).to_broadcast([128, 4, 64]), ALU.mult,
               ab(VT, VTn) + [gB[2]], ab(xpu))
            CP("pool", Xp[:, :, 1, :], ei.unsqueeze(2).to_broadcast([128, 4, 64]), [gB[2]], ab(xpu))
            D, DB, E, EB, dlast, elast = decay_unit(par, gates[:, 2, c, :], [gB[2]], 8, 4)
            Gb, GB = ps()
            G3 = Gb[:].rearrange("p (h l) -> p h l", h=4)
            mms = []
            for hd in range(4):
                b0 = (hd % 2) * 64
                mms.append(dict(out=G3[:, hd, :], lhsT=k_ap(hd // 2), rhs=q_ap(hd), start=True, stop=True))
            MMG(mms, [aB[KT + j * NG + g] for j in range(2)] + [aB[QT + j * NG + g] for j in range(4)], [GB])
            mu = (U_MT, SCR + 11)[par]
            MT = abf(mu).rearrange("p (h l) -> p h l", h=4)
            TT("dve", MT, D, G3, ALU.mult, DB + [GB], ab(mu))
            cu = U_CP + par
            CpT = abf(cu).rearrange("p (h l) -> p h l", h=4)
            for hd in range(4):
                TT("pool", CpT[:, hd, :], E[:, hd, :], q_ap(hd), ALU.mult, EB + [aB[QT + hd * NG + g]], ab(cu))
            Yb, YB = ps()
            Y4 = Yb[:].rearrange("p (t l) -> p t l", t=4)
            mms = []
            for hd in range(4):
                b0 = (hd % 2) * 64
                for s in range(2):
                    o = Y4[b0:b0 + 64, 2 * (hd // 2) + s, :]
                    mms.append(dict(out=o, lhsT=Xp[:, hd, s, :], rhs=MT[:, hd, :], start=True, stop=False))
                    mms.append(dict(out=o, lhsT=mlSb[:, hd // 2, s, :], rhs=CpT[:, hd, :], start=False, stop=True))
            MMG(mms, ab(xpu) + ab(mu) + ab(cu) + [stB[3]], [YB])
            TT("dve", Xpp.rearrange("p h s e -> p h (s e)"), Xp.rearrange("p h s e -> p h (s e)"),
               dlast.unsqueeze(2).to_broadcast([128, 4, 128]), ALU.mult, ab(xpu) + [smP[par][1]], ab(xppu))
            Sb_, SB_ = ps()
            S4 = Sb_[:, 0:256].rearrange("p (a s e) -> p a s e", a=2, s=2)
            mms = []
            for hd in range(4):
                b0 = (hd % 2) * 64
                for s in range(2):
                    mms.append(dict(out=S4[b0:b0 + 64, hd // 2, s, :], lhsT=ktok[:, hd * 64:(hd + 1) * 64],
                                    rhs=Xpp[:, hd, s, :], start=True, stop=True))
            MMG(mms, ab(KTK, VTn) + ab(xppu), [SB_])
            if os.environ.get("ML_DBG"):
                if "dbgS" not in st:
                    st["dbgS"] = nc.alloc_sbuf_tensor("dbgS", [128, 256], F32)
                    st["dbgSB"] = Buf()
                CP("dve", st["dbgS"][:], Sb_[:, 0:256], [SB_], [st["dbgSB"]])
            for hd in range(4):
                b0 = (hd % 2) * 64
                Sg = mlS[b0:b0 + 64, hd // 2, :, :]
                STT(Sg, Sg, elast[b0:b0 + 64, hd:hd + 1], S4[b0:b0 + 64, hd // 2, :, :], ALU.mult, ALU.add,
                    [stB[2], smP[par][2], SB_], [stB[2]])
                CP("act", mlSb[b0:b0 + 64, hd // 2, :, :], Sg, [stB[2]], [stB[3]])
            du = F_TMP + 4
            den = ff(du, 1).rearrange("p (a l) -> p a l", a=2)
            for a in range(2):
                ACT(den[:, a, :], Y4[:, 2 * a + 1, :], AF.Abs, [YB], fb(du))
            TS("dve", ff(du, 1), ff(du, 1), 1.0, ALU.max, fb(du), fb(du))
            RECIP(ff(du, 1), ff(du, 1), fb(du), fb(du))
            for a in range(2):
                u = HM + a * NG + g
                TT("dve", abf(u)[:, off:off + 128], Y4[:, 2 * a, :], den[:, a, :], ALU.mult, [YB] + fb(du), ab(u))
        wt, wbuf = W("w_in", l, 0, 1024, O_MO, 256)

        def gate(cix, g, u):
            pb, pbB = dense_fm(wt, wbuf, 8, cix * 128, lambda k, g_: u_ap(k, g_), lambda k, g_: u_b(k, g_), g)
            su = F_TMP + 2 * ((cix * NG + g) % 2)
            ACT(ff(su, 2), pb[:], AF.Sigmoid, [pbB], fb(su, 2))
            TT("dve", abf(u), abf(u), ff(su, 2), ALU.mult, ab(u) + fb(su, 2), ab(u))

        post_norm(HM, 2, "blk", vcol(l, V_MN, 2), gate=gate)
        wout_partial(l, 6, 2, lambda k, g_: abf(HM + k * NG + g_), lambda k, g_: ab(HM + k * NG + g_))

    def ffn_phase(l, q):
        AT = 0
        rmsnorm_h(vcol(l, V_N2, 8), u_ap, u_b)
        thirds = [(0, 8), (8, 8), (16, 6)]
        for (j0, nj) in thirds:
            for jj0 in range(0, nj, 4):
                njj = min(4, nj - jj0)
                wtg, wbg = W("wg", l, 0, 1024, (j0 + jj0) * 128, njj * 128)
                for jj in range(njj):
                    for g in range(NG):
                        pb, pbB = dense_fm(wtg, wbg, 8, jj * 128, lambda k, g_: u_ap(k, g_), lambda k, g_: u_b(k, g_), g)
                        u = AT + (jj0 + jj) * NG + g
                        ACT(abf(u), pb[:], AF.Silu, [pbB], ab(u))
                wtu, wbu = W("wu", l, 0, 1024, (j0 + jj0) * 128, njj * 128)
                for jj in range(njj):
                    for g in range(NG):
                        pb, pbB = dense_fm(wtu, wbu, 8, jj * 128, lambda k, g_: u_ap(k, g_), lambda k, g_: u_b(k, g_), g)
                        u = AT + (jj0 + jj) * NG + g
                        TT("dve", abf(u), abf(u), pb[:], ALU.mult, ab(u) + [pbB], ab(u))
            for half in range(2):
                wt, wbuf = W("wd", l, j0 * 128, nj * 128, half * 512, 512)
                for cc in range(4):
                    c = half * 4 + cc
                    for g in range(NG):
                        pb, pbB = dense_fm(wt, wbuf, nj, cc * 128, lambda k, g_: abf(AT + k * NG + g_),
                                           lambda k, g_: ab(AT + k * NG + g_), g)
                        add_to_h(c, g, pb, pbB)

    def ple_phase(l, q):
        PTU = 0
        for c in range(8):
            for g in range(NG):
                CP("dve" if (c + g) % 2 else "act", u_ap(c, g), hT[:, c, g * 512:(g + 1) * 512], [hB[c][g]], u_b(c, g))
        import os
        PS_ = int(os.environ.get("PLE_STEP", "9"))
        if PS_ <= 1:
            return
        for t in range(NT):
            su = (F_TMP + 4 + (t % 2)) if PS_ != 23 else (14 + (t % 2))
            ptile = ff(su, 1)
            if PS_ != 22:
                DMA("sp", ptile, p_d[l, q, t * 128:(t + 1) * 128, :], (), fb(su))
            else:
                MSET("dve", ptile, 1.0, fb(su))
            if PS_ in (21, 23):
                continue
            tb, tbB = ps()
            TRG([(tb[:, j * 128:(j + 1) * 128], ptile[:, j * 128:(j + 1) * 128], ident_f) for j in range(2)],
                fb(su) + [cB], [tbB])
            for j in range(2):
                u = PTU + j * NG + t // 4
                CP("act" if j else "dve", abf(u)[:, (t % 4) * 128:(t % 4) * 128 + 128], tb[:, j * 128:(j + 1) * 128],
                   [tbB], ab(u))
        if PS_ <= 2:
            return
        for half in range(2):
            wt1, wb1 = W("wpg", l, 0, 1024, half * 512, 512)
            wt2, wb2 = W("wpp", l, 0, 256, half * 512, 512, hold=1)
            for cc in range(4):
                c = half * 4 + cc
                for g in range(NG):
                    pa, paB = dense_fm(wt1, wb1, 8, cc * 128, lambda k, g_: u_ap(k, g_), lambda k, g_: u_b(k, g_), g)
                    pp, ppB = dense_fm(wt2, wb2, 2, cc * 128, lambda k, g_: abf(PTU + k * NG + g_),
                                       lambda k, g_: ab(PTU + k * NG + g_), g)
                    su = F_TMP + 2 * ((cc * NG + g) % 2)
                    ACT(ff(su, 2), pa[:], AF.Sigmoid, [paB], fb(su, 2))
                    TT("dve", ff(su, 2), ff(su, 2), pp[:], ALU.mult, fb(su, 2) + [ppB], fb(su, 2))
                    sl = slice(g * 512, (g + 1) * 512)
                    TT("dve", hT[:, c, sl], hT[:, c, sl], ff(su, 2), ALU.add, [hB[c][g]] + fb(su, 2), [hB[c][g]])

    def load_x(q):
        for t in range(NT):
            su = F_TMP + 4 * (t % 2)
            xt = ff(su, 4)
            DMA("sp", xt, x_d[q, t * 128:(t + 1) * 128, :], (), fb(su, 4))
            for hf in range(2):
                tb, tbB = ps()
                TRG([(tb[:, j * 128:(j + 1) * 128], xt[:, (hf * 4 + j) * 128:(hf * 4 + j + 1) * 128], ident_f)
                     for j in range(4)], fb(su, 4) + [cB], [tbB])
                CP("act" if hf else "dve", hT[:, hf * 4:hf * 4 + 4, t * 128:(t + 1) * 128],
                   tb[:].rearrange("p (j t) -> p j t", j=4), [tbB], [hB[hf * 4 + j][t // 4] for j in range(4)])

    outs = []

    def store_out(q, l):
        def dst(c, g):
            return hT[:, c, g * 512:(g + 1) * 512]

        def dstB(c, g):
            return [hB[c][g]]

        rmsnorm_h(vcol(l, V_NF, 8), dst, dstB)
        for t in range(NT):
            g = t // 4
            su = F_TMP + 4 * (t % 2)
            ot = ff(su, 4)
            for hf in range(2):
                tb, tbB = ps()
                TRG([(tb[:, j * 128:(j + 1) * 128], hT[:, hf * 4 + j, t * 128:(t + 1) * 128], ident_f) for j in range(4)],
                    [hB[hf * 4 + j][g] for j in range(4)] + [cB], [tbB])
                CP("act" if hf else "dve", ot[:, hf * 512:(hf + 1) * 512], tb[:], [tbB], fb(su, 4))
            outs.append(DMA("sp", out_d[q, t * 128:(t + 1) * 128, :], ot, fb(su, 4), ()))

    def dump_h():
        for c in range(8):
            outs.append(DMA("sp", dbg_d[:, c, :], hT[:, c, :], [hB[c][g] for g in range(NG)], ()))

    stop = int(dbg.split(":")[1]) if isinstance(dbg, str) and dbg.startswith("stop") else 99
    prologue()
    for q in range(NSEQ):
        if stop == 0:
            MSET("dve", hT[:], 1.0, [hB[c][g] for c in range(8) for g in range(NG)])
            dump_h()
            break
        load_x(q)
        if stop <= 1:
            dump_h()
            break
        for l in range(NL):
            rmsnorm_h(vcol(l, V_N1, 8), u_ap, u_b)
            if stop <= 2:
                break
            if dbg != "skipmix" and stop >= 10:
                ssd_phase(l, q)
                if stop >= 11:
                    diff_phase(l, q)
                if stop >= 12:
                    mlstm_phase(l, q)
            if stop in (10, 11, 12):
                break
            ffn_phase(l, q)
            if stop <= 3:
                break
            ple_phase(l, q)
            if stop <= 4:
                break
            if dbg and q == 0 and l == 0:
                dump_h()
        if stop < 99:
            dump_h()
            break
        store_out(q, NL - 1)
    if record_plan:
        return plan_out
    sch.emit(final_waits=outs)
    return nc


def _t5_bucket(dist):
    max_exact = 16
    d = np.maximum(dist, 1).astype(np.float32)
    large = max_exact + (np.log(d / max_exact) / math.log(128 / max_exact) * (32 - max_exact)).astype(np.int32)
    large = np.minimum(large, 31)
    return np.where(dist < max_exact, dist, large)


def _host_consts():
    r = np.arange(128)
    ident = np.eye(128, dtype=np.float32)
    tri = (r[:, None] <= r[None, :]).astype(np.float32)
    mask = np.where(r[:, None] <= r[None, :], 0.0, NEG).astype(np.float32)
    ones = np.ones((128, 128), np.float32)
    blk = (r[:, None] // 64 == r[None, :] // 64).astype(np.float32)
    sup = (r[:, None] > r[None, :]).astype(np.float32)
    return np.ascontiguousarray(np.concatenate([ident, tri, mask, ones, blk, sup], axis=1))


def _layout_small(inp, NL):
    f = lambda a: np.asarray(a, dtype=np.float32)
    fm = lambda v: f(v).reshape(-1, 128).T
    vecs, rows = [], []
    for l in range(NL):
        cw = f(inp["ssd_conv_w"][l])
        cols = [fm(inp["norm1_w"][l]), fm(inp["norm2_w"][l]), fm(inp["final_norm_w"]), fm(inp["ssd_conv_b"][l]),
                np.concatenate([fm(cw[k]) for k in range(4)], axis=1),
                fm(inp["ssd_norm_w"][l]),
                np.tile(f(inp["diff_norm_w"][l]), 2)[:, None], np.tile(f(inp["diff_norm_w"][l]), 2)[:, None],
                fm(inp["mlstm_norm_w"][l])]
        vecs.append(np.concatenate(cols, axis=1))
        lam = np.concatenate([f(inp["diff_lq1"][l]), f(inp["diff_lq2"][l]), f(inp["diff_lk1"][l]), f(inp["diff_lk2"][l])])
        rw = np.concatenate([f(inp["ssd_dt_bias"][l]), f(inp["ssd_a_log"][l]), f(inp["ssd_d"][l]),
                             f(inp["mlstm_i_bias"][l]), f(inp["mlstm_f_bias"][l]), lam,
                             f(inp["rel_bias"])[31, :]])
        rows.append(np.broadcast_to(rw[None, :], (128, rw.shape[0])))
    return np.ascontiguousarray(np.stack(vecs)), np.ascontiguousarray(np.stack(rows))


def _bias_tiles(rel_bias):
    rb = np.asarray(rel_bias, dtype=np.float32)
    k = np.arange(128)[:, None]
    c = np.arange(256)[None, :]
    dist = c - k
    bucket = _t5_bucket(np.maximum(dist, 0))
    g = rb[bucket]
    g = np.where((dist >= 0)[:, :, None], g, np.float32(NEG))
    return np.ascontiguousarray(np.transpose(g, (0, 2, 1)).reshape(128, 4 * 256))


_CACHE = {}


def _get_nc(S, NSEQ, NL, dbg=False):
    key = (S, NSEQ, NL, dbg)
    if key not in _CACHE:
        plan = build(S, NSEQ, NL, None, dbg)
        _CACHE[key] = build(S, NSEQ, NL, plan, dbg)
    return _CACHE[key]


def run(inp, n_cores, dbg=False):
    x = np.asarray(inp["x"], dtype=np.float32)
    B, S, _ = x.shape
    NL = inp["w_in"].shape[0]
    NSEQ = B // n_cores
    nc = _get_nc(S, NSEQ, NL, dbg)
    vecs, rows = _layout_small(inp, NL)
    consts = _host_consts()
    bt = _bias_tiles(inp["rel_bias"])
    p = np.asarray(inp["p"], dtype=np.float32)
    shared = {"consts": consts, "bt": bt, "vecs": vecs, "rows": rows}
    for k in ("w_in", "w_out", "w_ffn_gate", "w_ffn_up", "w_ffn_down", "ple_gate_w", "ple_proj_w"):
        shared[k] = np.ascontiguousarray(np.asarray(inp[k], dtype=np.float32))
    in_maps = []
    for c in range(n_cores):
        m = dict(shared)
        m["x"] = np.ascontiguousarray(x[c * NSEQ:(c + 1) * NSEQ])
        m["p"] = np.ascontiguousarray(p[:, c * NSEQ:(c + 1) * NSEQ])
        in_maps.append(m)
    res = run_bass_kernel_spmd(nc, in_maps, core_ids=list(range(n_cores)))
    out = np.concatenate([r["out"] for r in res.results], axis=0)
    if dbg:
        return out, [r["dbg"] for r in res.results]
    return out


def kernel(**inputs):
    return run(inputs, 8)
```
